# Optimizing a Trainium2 kernel written in Bass

```python
import math
import jax, jax.numpy as jnp
from jax import lax
import numpy as np

D_MODEL = 1024
BATCH = 8
SEQ = 2048
DEPTH = 4

CHUNK = 64
Q_BLOCK = 128
N_MIXERS = 2
HEAD_DIM = 128
N_MEM = 256
MEM_HEADS = 4
MEM_WIDTH = MEM_HEADS * HEAD_DIM
MIX_WIDTH = 2 * D_MODEL
CONV_WIDTH = MIX_WIDTH - MEM_WIDTH
CONV_KERNEL = 31
ATT_HEADS = (MIX_WIDTH - MEM_WIDTH) // HEAD_DIM
ATT_WIDTH = ATT_HEADS * HEAD_DIM
IDX_HEADS = 8
IDX_DIM = 64
TOPK_MAX = 256
ALPHA = (2 * DEPTH) ** 0.25
BETA = (8 * DEPTH) ** -0.25
LN_EPS = 1e-5
NEG = -1e30

A_SIZES = (2 * CONV_WIDTH, MEM_WIDTH, MIX_WIDTH)
B_SIZES = (ATT_WIDTH, HEAD_DIM, HEAD_DIM, IDX_HEADS * IDX_DIM, IDX_DIM, IDX_HEADS, MEM_WIDTH, MIX_WIDTH)
A_IN = sum(A_SIZES)
B_IN = sum(B_SIZES)

kernel_name = "hybrid_conformer_conv_dsa_memory_deepnorm"


def split_cols(h, sizes):
    return jnp.split(h, [int(c) for c in np.cumsum(sizes)[:-1]], axis=-1)


def layer_norm(x, g, b):
    xf = x.astype(jnp.float32)
    mu = xf.mean(-1, keepdims=True)
    var = jnp.square(xf - mu).mean(-1, keepdims=True)
    return ((xf - mu) * lax.rsqrt(var + LN_EPS) * g.astype(jnp.float32) + b.astype(jnp.float32)).astype(x.dtype)


def alibi_slopes(n):
    p = 2 ** int(math.floor(math.log2(n)))
    base = [2.0 ** (-8.0 * (i + 1) / p) for i in range(p)]
    extra = [2.0 ** (-4.0 * (2 * i + 1) / p) for i in range(n - p)]
    return jnp.asarray(base + extra, dtype=jnp.float32)


def mem_cross_attention(q, mem_n, w_mkv):
    B, T, _ = q.shape
    k, v = jnp.split(mem_n @ w_mkv, 2, axis=-1)
    q = q.reshape(B, T, MEM_HEADS, HEAD_DIM)
    k = k.reshape(B, -1, MEM_HEADS, HEAD_DIM)
    v = v.reshape(B, -1, MEM_HEADS, HEAD_DIM)
    s = jnp.einsum('bthd,bnhd->bhtn', q, k).astype(jnp.float32) * (HEAD_DIM ** -0.5)
    p = jax.nn.softmax(s, axis=-1).astype(v.dtype)
    return jnp.einsum('bhtn,bnhd->bthd', p, v).reshape(B, T, MEM_WIDTH)


def conformer_conv(u, conv_w, conv_b, g, b):
    a, gl = jnp.split(u, 2, axis=-1)
    h = a * jax.nn.sigmoid(gl)
    h = jnp.pad(h, ((0, 0), (CONV_KERNEL - 1, 0), (0, 0)))
    h = lax.conv_general_dilated(h, conv_w[:, None, :].astype(h.dtype), (1,), 'VALID',
                                 dimension_numbers=('NWC', 'WIO', 'NWC'),
                                 feature_group_count=CONV_WIDTH) + conv_b
    return jax.nn.silu(layer_norm(h, g, b))


def dsa_attention(q, k, v, q_idx, k_idx, w_idx):
    B, T = q.shape[:2]
    topk = min(TOPK_MAX, T // 4)
    nb = T // Q_BLOCK
    slopes = alibi_slopes(ATT_HEADS)
    key_chunk = jnp.arange(T, dtype=jnp.int32) // CHUNK
    scale = HEAD_DIM ** -0.5

    def to_blocks(a):
        return jnp.moveaxis(a.reshape(B, nb, Q_BLOCK, *a.shape[2:]), 1, 0)

    def block(args):
        qb, qib, wb, start = args
        qpos = start + jnp.arange(Q_BLOCK, dtype=jnp.int32)
        qchunk = qpos // CHUNK
        admiss = key_chunk[None, :] <= qchunk[:, None]
        iscore = jax.nn.relu(jnp.einsum('bqhd,bsd->bqhs', qib, k_idx).astype(jnp.float32))
        iscore = jnp.einsum('bqh,bqhs->bqs', wb.astype(jnp.float32), iscore)
        iscore = jnp.where(admiss[None], iscore, NEG)
        _, sel = lax.top_k(iscore, topk)
        valid = key_chunk[sel] <= qchunk[None, :, None]
        kg = jax.vmap(lambda kb, ib: kb[ib])(k, sel)
        vg = jax.vmap(lambda vb, ib: vb[ib])(v, sel)
        dist = jnp.abs(qpos[None, :, None] - sel).astype(jnp.float32)
        logits = jnp.einsum('bqhd,bqkd->bqhk', qb, kg).astype(jnp.float32) * scale
        logits = logits - slopes[None, None, :, None] * dist[:, :, None, :]
        logits = jnp.where(valid[:, :, None, :], logits, NEG)
        p = jax.nn.softmax(logits, axis=-1).astype(vg.dtype)
        return jnp.einsum('bqhk,bqkd->bqhd', p, vg)

    starts = jnp.arange(nb, dtype=jnp.int32) * Q_BLOCK
    out = lax.map(block, (to_blocks(q), to_blocks(q_idx), to_blocks(w_idx), starts))
    return jnp.moveaxis(out, 0, 1).reshape(B, T, ATT_WIDTH)


def setup_inputs(seed: int = 0) -> dict:
    key = jax.random.key(seed)
    ks = jax.random.split(key, 24)
    n_a = (DEPTH + 1) // 2
    n_b = DEPTH // 2
    nrm = lambda k, shape, s: jax.random.normal(k, shape, jnp.float32) * s
    return {
        "x": nrm(ks[0], (BATCH, SEQ, D_MODEL), 1.0),
        "mem": nrm(ks[1], (BATCH, N_MEM, D_MODEL), 1.0),
        "mem_ln_g": 1.0 + nrm(ks[2], (D_MODEL,), 0.05),
        "mem_ln_b": nrm(ks[3], (D_MODEL,), 0.01),
        "a_w_in": nrm(ks[4], (n_a, D_MODEL, A_IN), D_MODEL ** -0.5),
        "a_conv_w": nrm(ks[5], (n_a, CONV_KERNEL, CONV_WIDTH), CONV_KERNEL ** -0.5),
        "a_conv_b": nrm(ks[6], (n_a, CONV_WIDTH), 0.01),
        "a_ln_g": 1.0 + nrm(ks[7], (n_a, CONV_WIDTH), 0.05),
        "a_ln_b": nrm(ks[8], (n_a, CONV_WIDTH), 0.01),
        "a_w_mkv": nrm(ks[9], (n_a, D_MODEL, 2 * MEM_WIDTH), D_MODEL ** -0.5),
        "a_w_out": nrm(ks[10], (n_a, MIX_WIDTH, D_MODEL), BETA * MIX_WIDTH ** -0.5),
        "a_post_g": 1.0 + nrm(ks[11], (n_a, D_MODEL), 0.05),
        "a_post_b": nrm(ks[12], (n_a, D_MODEL), 0.01),
        "b_w_in": nrm(ks[13], (n_b, D_MODEL, B_IN), D_MODEL ** -0.5),
        "b_w_mkv": nrm(ks[14], (n_b, D_MODEL, 2 * MEM_WIDTH), D_MODEL ** -0.5),
        "b_w_out": nrm(ks[15], (n_b, MIX_WIDTH, D_MODEL), BETA * MIX_WIDTH ** -0.5),
        "b_post_g": 1.0 + nrm(ks[16], (n_b, D_MODEL), 0.05),
        "b_post_b": nrm(ks[17], (n_b, D_MODEL), 0.01),
    }


def reference(x, mem, mem_ln_g, mem_ln_b, a_w_in, a_conv_w, a_conv_b, a_ln_g, a_ln_b,
              a_w_mkv, a_w_out, a_post_g, a_post_b, b_w_in, b_w_mkv, b_w_out,
              b_post_g, b_post_b):
    B, T, _ = x.shape
    mem_n = layer_norm(mem, mem_ln_g, mem_ln_b)
    for i in range(DEPTH):
        j = i // N_MIXERS
        if i % N_MIXERS == 0:
            u, qm, gate = split_cols(x @ a_w_in[j], A_SIZES)
            y_mix = conformer_conv(u, a_conv_w[j], a_conv_b[j], a_ln_g[j], a_ln_b[j])
            w_mkv, w_out, pg, pb = a_w_mkv[j], a_w_out[j], a_post_g[j], a_post_b[j]
        else:
            q, k, v, qi, ki, wi, qm, gate = split_cols(x @ b_w_in[j], B_SIZES)
            y_mix = dsa_attention(q.reshape(B, T, ATT_HEADS, HEAD_DIM), k, v,
                                  qi.reshape(B, T, IDX_HEADS, IDX_DIM), ki, wi)
            w_mkv, w_out, pg, pb = b_w_mkv[j], b_w_out[j], b_post_g[j], b_post_b[j]
        y_mem = mem_cross_attention(qm, mem_n, w_mkv)
        y = jnp.concatenate([y_mix, y_mem], axis=-1) * jax.nn.silu(gate)
        x = layer_norm(ALPHA * x + y @ w_out, pg, pb)
    return x
```

```python
import numpy as np
from contextlib import ExitStack
import concourse.bass as bass
import concourse.mybir as mybir
from concourse.bass_utils import run_bass_kernel_spmd

F32 = mybir.dt.float32
BF16 = mybir.dt.bfloat16
AF = mybir.ActivationFunctionType
ALU = mybir.AluOpType
AX = mybir.AxisListType

D_MODEL = 1024
SEQ = 2048
DEPTH = 4
N_MEM = 256
HD = 128
CONV_W = 1536
CONV_K = 31
ATT_H = 12
IDX_H = 8
IDX_D = 64
TOPK = 256
ALPHA = (2 * DEPTH) ** 0.25
LN_EPS = 1e-5
SCALE = HD ** -0.5
A_IN = 5632
B_IN = 4936
NBISECT = 22
DEBUG = False
MASKNEG = -30000.0


def _alibi_slopes(n):
    import math
    p = 2 ** int(math.floor(math.log2(n)))
    base = [2.0 ** (-8.0 * (i + 1) / p) for i in range(p)]
    extra = [2.0 ** (-4.0 * (2 * i + 1) / p) for i in range(n - p)]
    return base + extra


class _Op:
    __slots__ = ("eng", "fn", "deps", "dma", "sig", "cnt", "waits", "clock", "gid")


class Sched:
    ENG = ("pe", "act", "dve", "pool", "sp")

    def __init__(self):
        self.ops = []
        self.lastw = {}
        self.readers = {}
        self.streams = {}
        self.last_on = {}
        self.pending_dma = []

    def op(self, eng, fn, r=(), w=(), dma=None, extra=()):
        o = _Op()
        o.eng = eng; o.fn = fn; o.dma = dma; o.sig = False; o.gid = len(self.ops)
        o.cnt = 0; o.clock = None; o.waits = ()
        deps = {}

        def add(d, kind):
            if d is None or d is o:
                return
            if d.dma is None and dma is None and d.eng == eng:
                if eng == "pe":
                    return
                if kind == "war":
                    return
            deps[d.gid] = d

        for k in r:
            add(self.lastw.get(k), "raw")
            if isinstance(k, tuple) and k[0] == "ps":
                rd = self.readers.get(k)
                if rd:
                    for x in rd.values():
                        if x.eng != eng:
                            add(x, "raw")
        for k in w:
            add(self.lastw.get(k), "waw")
            rd = self.readers.get(k)
            if rd:
                for x in rd.values():
                    add(x, "war")
        for d in extra:
            add(d, "raw")
        for k in w:
            self.lastw[k] = o
            self.readers[k] = {}
        for k in r:
            rd = self.readers.setdefault(k, {})
            rd[("dma", o.gid) if dma is not None else eng] = o
        o.deps = list(deps.values())
        self.ops.append(o)
        self.last_on[eng] = o
        if dma is not None:
            self.streams[dma] = self.streams.get(dma, 0) + 1
            o.cnt = self.streams[dma] * 16
            self.pending_dma.append(o)
        return o

    def barrier(self):
        last = [self.last_on[e] for e in self.ENG if e in self.last_on]
        dmas = list(self.pending_dma)
        self.pending_dma = []
        for e in self.ENG:
            ex = [d for d in last if d.eng != e or d.dma is not None] + dmas
            self.op(e, (lambda en: en.nop()), extra=ex)

    def finalize(self):
        for o in self.ops:
            for d in o.deps:
                d.sig = True
        cnt = {e: 0 for e in self.ENG}
        for o in self.ops:
            if o.dma is None and o.sig:
                cnt[o.eng] += 1
                o.cnt = cnt[o.eng]
        eclock = {e: {} for e in self.ENG}
        for o in self.ops:
            ck = eclock[o.eng]
            waits = {}
            for d in sorted(o.deps, key=lambda t: t.gid):
                key = d.dma if d.dma is not None else d.eng
                if ck.get(key, 0) >= d.cnt:
                    continue
                waits[key] = max(waits.get(key, 0), d.cnt)
                for k2, v2 in d.clock.items():
                    if ck.get(k2, 0) < v2:
                        ck[k2] = v2
            o.waits = tuple(waits.items())
            if o.dma is not None:
                c2 = dict(ck); c2[o.dma] = max(c2.get(o.dma, 0), o.cnt); o.clock = c2
            elif o.sig:
                c2 = dict(ck); c2[o.eng] = o.cnt; o.clock = c2
        self.byeng = {e: [o for o in self.ops if o.eng == e] for e in self.ENG}

    def emit(self, nc, stack):
        self.finalize()
        sems = {}
        for key in list(self.ENG) + list(self.streams.keys()):
            sems[key] = stack.enter_context(nc.semaphore("s_" + str(key)))
        streams = self.streams
        byeng = self.byeng

        def runner(engname):
            def f(e):
                for o in byeng[engname]:
                    for key, val in o.waits:
                        e.wait_ge(sems[key], val)
                    ins = o.fn(e)
                    if o.dma is not None:
                        ins.then_inc(sems[o.dma], 16)
                    elif o.sig:
                        ins.then_inc(sems[o.eng], 1)
                if engname == "sp":
                    for key, n in streams.items():
                        e.wait_ge(sems[key], 16 * n)
            return f

        with nc.Block() as block:
            block.tensor(runner("pe"))
            block.scalar(runner("act"))
            block.vector(runner("dve"))
            block.gpsimd(runner("pool"))
            block.sync(runner("sp"))


class Prog:
    def __init__(self, layers, final_to_out=True):
        self.layers = list(layers)
        self.S = Sched()
        self.nc = bass.Bass("TRN2", target_bir_lowering=False)
        self.ps_rr = 0
        self.uid = 0

    def nm(self, name):
        self.ncnt = getattr(self, "ncnt", 0) + 1
        return "%s_%d" % (name, self.ncnt)

    def MM(self, out, lhsT, rhs, start, stop, r, w):
        return self.S.op("pe", lambda e: e.matmul(out, lhsT, rhs, start=start, stop=stop), r=r, w=w)

    def TR(self, out, in_, ident, r, w):
        return self.S.op("pe", lambda e: e.transpose(out, in_, ident), r=r, w=w)

    def ACT(self, out, in_, func, r, w, bias=None, scale=None, accum=None):
        kw = {}
        if bias is not None:
            kw["bias"] = bias
        if scale is not None:
            kw["scale"] = scale
        if accum is not None:
            kw["accum_out"] = accum
        return self.S.op("act", lambda e: e.activation(out=out, in_=in_, func=func, **kw), r=r, w=w)

    def TS(self, eng, out, in0, s1, s2, op0, op1, r, w, accum=None):
        kw = {}
        if op1 is not None:
            kw["op1"] = op1
        if accum is not None:
            kw["accum_out"] = accum
        return self.S.op(eng, lambda e: e.tensor_scalar(out=out, in0=in0, scalar1=s1, scalar2=s2, op0=op0, **kw), r=r, w=w)

    def TT(self, eng, out, in0, in1, op, r, w):
        return self.S.op(eng, lambda e: e.tensor_tensor(out=out, in0=in0, in1=in1, op=op), r=r, w=w)

    def STT(self, eng, out, in0, scalar, in1, op0, op1, r, w):
        return self.S.op(eng, lambda e: e.scalar_tensor_tensor(out=out, in0=in0, scalar=scalar, in1=in1, op0=op0, op1=op1), r=r, w=w)

    def CP(self, eng, out, in_, r, w):
        if eng == "act":
            return self.S.op("act", lambda e: e.copy(out=out, in_=in_), r=r, w=w)
        return self.S.op(eng, lambda e: e.tensor_copy(out=out, in_=in_), r=r, w=w)

    def MEMSET(self, eng, ap, val, w):
        return self.S.op(eng, lambda e: e.memset(ap, val), w=w)

    def DMA(self, out, in_, stream, r, w):
        return self.S.op("sp", lambda e: e.dma_start(out=out, in_=in_), r=r, w=w, dma=stream)

    def psum(self):
        i = self.ps_rr
        self.ps_rr = (self.ps_rr + 1) % 8
        return i

    def wload(self, pieces, shape):
        s = self.w_rr % 2
        b = self.wb_rr % 2
        self.w_rr += 1
        self.wb_rr += 1
        a, bb = shape[1], shape[2]
        n = a * bb
        assert n <= 2048
        stv = self.wst[s][:, 0:n].rearrange("p (a b) -> p a b", a=a)
        bfv = self.wbf[b][:, 0:n].rearrange("p (a b) -> p a b", a=a)
        for dst_fn, src in pieces:
            self.DMA(dst_fn(stv), src, "wst%d" % s, r=[], w=[("wst", s)])
        self.CP("act", bfv, stv, r=[("wst", s)], w=[("wbf", b)])
        return bfv, ("wbf", b)

    def build(self):
        nc = self.nc
        S = self.S
        L = self.layers
        dt = nc.dram_tensor
        self.x_in = dt("x", [SEQ, D_MODEL], F32, kind="ExternalInput").ap()
        self.mem_in = dt("mem", [N_MEM, D_MODEL], F32, kind="ExternalInput").ap()
        self.memgb = dt("memgb", [128, 2 * D_MODEL], F32, kind="ExternalInput").ap()
        self.c_ident = dt("c_ident", [128, 128], F32, kind="ExternalInput").ap()
        self.c_dtab = dt("c_dtab", [128, SEQ], F32, kind="ExternalInput").ap()
        self.c_madm = dt("c_madm", [128, 256], F32, kind="ExternalInput").ap()
        self.a_w_in = dt("a_w_in", [2, D_MODEL, A_IN], F32, kind="ExternalInput").ap()
        self.a_sp = dt("a_sp", [2, 128, 36 + 12 * CONV_K], F32, kind="ExternalInput").ap()
        self.a_w_mkv = dt("a_w_mkv", [2, D_MODEL, 1024], F32, kind="ExternalInput").ap()
        self.a_w_out = dt("a_w_out", [2, 2048, D_MODEL], F32, kind="ExternalInput").ap()
        self.a_pgb = dt("a_pgb", [2, 128, 2 * D_MODEL], F32, kind="ExternalInput").ap()
        self.b_w_in = dt("b_w_in", [2, D_MODEL, B_IN], F32, kind="ExternalInput").ap()
        self.b_w_mkv = dt("b_w_mkv", [2, D_MODEL, 1024], F32, kind="ExternalInput").ap()
        self.b_w_out = dt("b_w_out", [2, 2048, D_MODEL], F32, kind="ExternalInput").ap()
        self.b_pgb = dt("b_pgb", [2, 128, 2 * D_MODEL], F32, kind="ExternalInput").ap()
        self.out = dt("out", [SEQ, D_MODEL], F32, kind="ExternalOutput").ap()
        self.dbg = dt("dbg", [128, 16 * SEQ], BF16, kind="ExternalOutput").ap() if DEBUG else None
        if DEBUG:
            self.dbg_k = dt("dbg_k", [128, 4 * 256], BF16, kind="ExternalOutput").ap()
            self.dbg_v = dt("dbg_v", [128, 2 * 512], BF16, kind="ExternalOutput").ap()
            self.dbg_m = dt("dbg_m", [128, 8 * 256], BF16, kind="ExternalOutput").ap()
        self.xs = [dt("xs0", [SEQ, D_MODEL], F32).ap(), dt("xs1", [SEQ, D_MODEL], F32).ap()]

        with ExitStack() as st:
            sb = lambda name, shape, dtype: st.enter_context(nc.sbuf_tensor(self.nm(name), shape, dtype))
            self.PS = [st.enter_context(nc.psum_tensor("ps%d" % i, [128, 512], F32)) for i in range(8)]
            self.identf = sb("identf", [128, 128], F32)
            self.ident = sb("ident", [128, 128], BF16)
            self.ones = sb("ones", [128, 128], BF16)
            self.memT = sb("memT", [128, 8, N_MEM], BF16)
            self.xT = sb("xT", [128, 8, SEQ], BF16)
            self.QY = sb("QY", [128, 16, SEQ], BF16)
            self.wst = [sb("wst%d" % i, [128, 2048], F32) for i in range(2)]
            self.wbf = [sb("wbf%d" % i, [128, 2048], BF16) for i in range(2)]
            self.kTm = sb("kTm", [128, 4, N_MEM], BF16)
            self.vm = sb("vm", [128, 2, 512], BF16)
            self.sm = sb("sm", [128, 64], F32)
            self.w_rr = 0
            self.wb_rr = 0

            self.DMA(self.identf[:], self.c_ident[:, :], "cst", r=[], w=["identf"])
            self.CP("dve", self.ident[:], self.identf[:], r=["identf"], w=["ident"])
            self.MEMSET("dve", self.ones[:], 1.0, w=["ones"])

            with ExitStack() as st0:
                sb0 = lambda name, shape, dtype: st0.enter_context(nc.sbuf_tensor(self.nm(name), shape, dtype))
                self.xio = [sb0("xio%d" % i, [128, D_MODEL], F32) for i in range(2)]
                self.xb = [sb0("xb%d" % i, [128, D_MODEL], BF16) for i in range(2)]
                self.pgb = sb0("pgb", [128, 2 * D_MODEL], F32)
                self.lnst = sb0("lnst", [128, 12], F32)
                self.lnag = sb0("lnag", [128, 4], F32)
                self.DMA(self.pgb[:], self.memgb[:, :], "pgb", r=[], w=["pgb"])
                for t in range(2):
                    s = t % 2
                    self.DMA(self.xio[s][:], self.mem_in[t * 128:(t + 1) * 128, :], "xio%d" % s, r=[], w=[("xio", s)])
                    self.layernorm_tile(s)
                    self.to_T(s, self.memT, t, ("memT", t))
                for t in range(16):
                    s = t % 2
                    self.DMA(self.xio[s][:], self.x_in[t * 128:(t + 1) * 128, :], "xio%d" % s, r=[], w=[("xio", s)])
                    self.CP("act", self.xb[s][:], self.xio[s][:], r=[("xio", s)], w=[("xb", s)])
                    self.to_T(s, self.xT, t, ("xT", t // 4))
            S.barrier()

            xsrc = self.x_in
            for li, layer in enumerate(L):
                j = layer // 2
                last = (li == len(L) - 1)
                xdst = self.out if last else self.xs[li % 2]
                if layer % 2 == 0:
                    self.layer_A(j)
                    wout, pgbsrc, wgate, goff = self.a_w_out[j], self.a_pgb[j], self.a_w_in[j], 3584
                else:
                    self.layer_B(j)
                    wout, pgbsrc, wgate, goff = self.b_w_out[j], self.b_pgb[j], self.b_w_in[j], 2888
                S.barrier()
                if DEBUG and li == 0:
                    self.DMA(self.dbg[:, :], self.QY[:, :, :].rearrange("p f t -> p (f t)"), "dbg", r=[("QY", f, t) for f in range(16) for t in range(16)], w=[])
                    self.DMA(self.dbg_k[:, :], self.kTm[:, :, :].rearrange("p f t -> p (f t)"), "dbg", r=[("kTm", h) for h in range(4)], w=[])
                    self.DMA(self.dbg_v[:, :], self.vm[:, :, :].rearrange("p f t -> p (f t)"), "dbg", r=[("vm", 0, 0)], w=[])
                    self.DMA(self.dbg_m[:, :], self.memT[:, :, :].rearrange("p f t -> p (f t)"), "dbg", r=[("memT", 0)], w=[])
                    S.barrier()
                self.gate_out(wgate, goff, wout, pgbsrc, xsrc, xdst, last, li)
                S.barrier()
                xsrc = xdst
            S.emit(nc, st)
        return nc

    def layernorm_tile(self, s):
        xk = ("xio", s)
        x = self.xio[s]
        lnst, lnag, pgb, xb = self.lnst, self.lnag, self.pgb, self.xb[s]
        S = self.S
        for c in range(2):
            S.op("dve", (lambda e, c=c, lnst=lnst, x=x: e.bn_stats(lnst[:, c * 6:(c + 1) * 6], x[:, c * 512:(c + 1) * 512])),
                 r=[xk], w=[("lnst", c)])
        S.op("dve", (lambda e, lnst=lnst, lnag=lnag: e.bn_aggr(lnag[:, 0:2], lnst[:, :])), r=[("lnst", 0), ("lnst", 1)], w=["lnag"])
        self.TS("dve", lnag[:, 2:3], lnag[:, 1:2], LN_EPS, None, ALU.add, None, r=["lnag"], w=["lnrs0"])
        S.op("act", (lambda e, lnag=lnag: e.sqrt(lnag[:, 2:3], lnag[:, 2:3])), r=["lnrs0"], w=["lnrs0"])
        S.op("dve", (lambda e, lnag=lnag: e.reciprocal(lnag[:, 3:4], lnag[:, 2:3])), r=["lnrs0"], w=["lnrs"])
        self.TS("dve", x[:], x[:], lnag[:, 0:1], lnag[:, 3:4], ALU.subtract, ALU.mult, r=[xk, "lnag", "lnrs"], w=[xk])
        self.TT("dve", x[:], x[:], pgb[:, 0:D_MODEL], ALU.mult, r=[xk, "pgb"], w=[xk])
        self.TT("dve", x[:], x[:], pgb[:, D_MODEL:2 * D_MODEL], ALU.add, r=[xk, "pgb"], w=[xk])
        self.CP("act", xb[:], x[:], r=[xk], w=[("xb", s)])

    def to_T(self, s, dstT, t, dkey):
        pi = self.psum()
        pk = ("ps", pi)
        pv = self.PS[pi][:].bitcast(BF16)
        for k in range(8):
            self.TR(pv[:, k * 128:(k + 1) * 128], self.xb[s][:, k * 128:(k + 1) * 128], self.ident[:],
                    r=[("xb", s), "ident"], w=[pk])
        self.CP("dve", dstT[:, :, t * 128:(t + 1) * 128], pv.rearrange("p (k t) -> p k t", k=8), r=[pk], w=[dkey])

    def proj_fm(self, wbfv, wkey, ct, tb, evac):
        pi = self.psum()
        pk = ("ps", pi)
        for k in range(8):
            self.MM(self.PS[pi][:, :], wbfv[:, k, ct * 128:(ct + 1) * 128], self.xT[:, k, tb * 512:(tb + 1) * 512],
                    k == 0, k == 7, r=[wkey, ("xT", tb)], w=[pk])
        evac(pi, pk)

    def win_pieces(self, w2d, cols):
        pieces = []
        off = 0
        for (c0, n) in cols:
            src = w2d[:, c0:c0 + n].rearrange("(k p) c -> p k c", p=128)
            pieces.append(((lambda v, off=off, n=n: v[:, :, off:off + n]), src))
            off += n
        return pieces, off

    def mem_kv(self, wmkv):
        for g in range(2):
            pieces, n = self.win_pieces(wmkv, [(g * 256, 256)])
            wv, wk = self.wload(pieces, [128, 8, 256])
            for ct in range(2):
                h = g * 2 + ct
                pi = self.psum(); pk = ("ps", pi)
                for k in range(8):
                    self.MM(self.PS[pi][:, 0:N_MEM], wv[:, k, ct * 128:(ct + 1) * 128], self.memT[:, k, :], k == 0, k == 7,
                            r=[wk, ("memT", 0), ("memT", 1)], w=[pk])
                self.CP("act", self.kTm[:, h, :], self.PS[pi][:, 0:N_MEM], r=[pk], w=[("kTm", h)])
        for g in range(2):
            pieces, n = self.win_pieces(wmkv, [(512 + g * 256, 256)])
            wv, wk = self.wload(pieces, [128, 8, 256])
            for c in range(2):
                pi = self.psum(); pk = ("ps", pi)
                for k in range(8):
                    self.MM(self.PS[pi][:, 0:256], self.memT[:, k, c * 128:(c + 1) * 128], wv[:, k, :], k == 0, k == 7,
                            r=[wk, ("memT", c)], w=[pk])
                self.CP("act", self.vm[:, c, g * 256:(g + 1) * 256], self.PS[pi][:, 0:256], r=[pk], w=[("vm", c, g)])

    def _ak(self, slot, scr):
        sm = scr["sm"]
        sid = (scr["id"], slot)
        c0s = 16 + 8 * slot
        cols = [sm[:, c0s + i:c0s + i + 1] for i in range(4)]
        keys = [("smc", sid, i) for i in range(4)]
        return sid, cols, keys

    def at_qk(self, job, slot, scr):
        qT, qkeys, kT, kkeys, nk, bias = job["qT"], job["qkeys"], job["kT"], job["kkeys"], job["nk"], job["bias"]
        sid, cols, keys = self._ak(slot, scr)
        nch = (nk + 511) // 512
        if bias is not None:
            Dsl, ch, M, mkeys = bias
            z = scr["z"][slot]
            zk = ("z", sid)
            for c in range(nch):
                c0 = c * 512
                n = min(512, nk - c0)
                pi = self.psum(); pk = ("ps", pi)
                self.MM(self.PS[pi][:, 0:n], self.ident[:, :], M[:, c0:c0 + n], True, False, r=["ident"] + list(mkeys), w=[pk])
                self.MM(self.PS[pi][:, 0:n], qT, kT[:, c0:c0 + n], False, True, r=list(qkeys) + list(kkeys), w=[pk])
                self.STT("dve", z[:, c0:c0 + n], Dsl[:, c0:c0 + n], ch, self.PS[pi][:, 0:n], ALU.mult, ALU.add,
                         r=[pk, "dtab"], w=[zk])
        else:
            pi = self.psum(); pk = ("ps", pi)
            self.MM(self.PS[pi][:, 0:nk], qT, kT[:, 0:nk], True, True, r=list(qkeys) + list(kkeys), w=[pk])
            job["_ps"] = pi

    def at_max(self, job, slot, scr):
        nk = job["nk"]
        sid, (cmx, cnm, crs, cri), (kmx, knm, krs, kri) = self._ak(slot, scr)
        p = scr["p"][slot]
        pkey = ("p", sid)
        if job["bias"] is not None:
            src, sk = scr["z"][slot][:, 0:nk], ("z", sid)
        else:
            src, sk = self.PS[job["_ps"]][:, 0:nk], ("ps", job["_ps"])
        self.TS("dve", p[:, 0:nk], src, 0.0, None, ALU.add, ALU.max, r=[sk], w=[pkey, kmx], accum=cmx)
        self.TS("dve", cnm, cmx, -SCALE, None, ALU.mult, None, r=[kmx], w=[knm])

    def at_exp(self, job, slot, scr):
        nk = job["nk"]
        sid, (cmx, cnm, crs, cri), (kmx, knm, krs, kri) = self._ak(slot, scr)
        p = scr["p"][slot]
        pkey = ("p", sid)
        if job["bias"] is not None:
            src, sk = scr["z"][slot][:, 0:nk], ("z", sid)
        else:
            src, sk = self.PS[job["_ps"]][:, 0:nk], ("ps", job["_ps"])
        self.ACT(p[:, 0:nk], src, AF.Exp, r=[sk, knm], w=[pkey, krs], bias=cnm, scale=SCALE, accum=crs)

    def at_tr(self, job, slot, scr):
        nk = job["nk"]
        sid, cols, keys = self._ak(slot, scr)
        p = scr["p"][slot]
        pkey = ("p", sid)
        nc128 = nk // 128
        banks = []
        for b0 in range(0, nc128, 8):
            nb = min(8, nc128 - b0)
            pi = self.psum(); pk = ("ps", pi)
            pv = self.PS[pi][:].bitcast(BF16)
            for c in range(nb):
                self.TR(pv[:, c * 128:(c + 1) * 128], p[:, (b0 + c) * 128:(b0 + c + 1) * 128], self.ident[:],
                        r=[pkey, "ident"], w=[pk])
            banks.append((b0, nb, pi))
        job["_tb"] = banks

    def at_evac(self, job, slot, scr):
        pT = scr["pT"]
        ptk = ("pT", scr["id"])
        for (b0, nb, pi) in job["_tb"]:
            pv = self.PS[pi][:].bitcast(BF16)
            self.CP("act", pT[:, b0:b0 + nb, :], pv[:, 0:nb * 128].rearrange("p (c t) -> p c t", c=nb), r=[("ps", pi)], w=[(ptk, b0)])

    def at_pv(self, job, slot, scr):
        nk, vfn, vkeys = job["nk"], job["vfn"], job["vkeys"]
        pT = scr["pT"]
        ptk = ("pT", scr["id"])
        nc128 = nk // 128
        pi = self.psum(); pk = ("ps", pi)
        for c in range(nc128):
            self.MM(self.PS[pi][:, 0:128], pT[:, c, :], vfn(c), c == 0, c == nc128 - 1,
                    r=[(ptk, (c // 8) * 8)] + list(vkeys), w=[pk])
        job["_pv"] = pi

    def at_fin(self, job, slot, scr):
        sid, (cmx, cnm, crs, cri), (kmx, knm, krs, kri) = self._ak(slot, scr)
        self.S.op("dve", (lambda e, cri=cri, crs=crs: e.reciprocal(cri, crs)), r=[krs], w=[kri])
        pi = job["_pv"]
        self.ACT(job["ydst"], self.PS[pi][:, 0:128], AF.Identity, r=[("ps", pi), kri], w=[job["ydkey"]], scale=cri)
        if job.get("post") is not None:
            job["post"]()

    def attn_pipeline_gen(self, jobs, scr):
        n = len(jobs)
        for i in range(n + 1):
            cur = jobs[i] if i < n else None
            prv = jobs[i - 1] if i >= 1 else None
            cs, ps_ = i % 2, (i - 1) % 2
            if cur is not None:
                self.at_qk(cur, cs, scr)
            if prv is not None:
                self.at_tr(prv, ps_, scr)
                self.at_evac(prv, ps_, scr)
            if cur is not None:
                self.at_max(cur, cs, scr)
                self.at_exp(cur, cs, scr)
            if prv is not None:
                self.at_pv(prv, ps_, scr)
                self.at_fin(prv, ps_, scr)
            yield

    def attn_pipeline(self, jobs, scr, side=None, per=1):
        for _ in self.attn_pipeline_gen(jobs, scr):
            if side is not None:
                for _k in range(per):
                    next(side, None)

    def ytok_to_QY(self, ytok, ykeys, f0, nf, qt):
        for b0 in range(0, nf, 8):
            nb = min(8, nf - b0)
            pi = self.psum(); pk = ("ps", pi)
            pv = self.PS[pi][:].bitcast(BF16)
            for c in range(nb):
                self.TR(pv[:, c * 128:(c + 1) * 128], ytok[:, (b0 + c) * 128:(b0 + c + 1) * 128], self.ident[:],
                        r=list(ykeys) + ["ident"], w=[pk])
            self.CP("act", self.QY[:, f0 + b0:f0 + b0 + nb, qt * 128:(qt + 1) * 128],
                    pv[:, 0:nb * 128].rearrange("p (c t) -> p c t", c=nb), r=[pk],
                    w=[("QY", f0 + b0 + c, qt) for c in range(nb)])

    def mem_attention(self, scr, ytok):
        jobs = []
        for qt in range(16):
            for h in range(4):
                job = dict(qT=self.QY[:, 12 + h, qt * 128:(qt + 1) * 128], qkeys=[("QY", 12 + h, qt)],
                           kT=self.kTm[:, h, :], kkeys=[("kTm", h)], nk=N_MEM,
                           vfn=(lambda c, h=h: self.vm[:, c, h * 128:(h + 1) * 128]),
                           vkeys=[("vm", 0, 0), ("vm", 0, 1), ("vm", 1, 0), ("vm", 1, 1)],
                           bias=None, ydst=ytok[:, h * 128:(h + 1) * 128], ydkey=("ytok", h), post=None)
                if h == 3:
                    job["post"] = (lambda qt=qt: self.ytok_to_QY(ytok, [("ytok", hh) for hh in range(4)], 12, 4, qt))
                jobs.append(job)
        return self.attn_pipeline_gen(jobs, scr)

    def layer_A(self, j):
        nc = self.nc
        S = self.S
        w_in = self.a_w_in[j]
        with ExitStack() as st:
            sb = lambda name, shape, dtype: st.enter_context(nc.sbuf_tensor(self.nm(name), shape, dtype))
            hbuf = [sb("h%d" % i, [128, 2080], BF16) for i in range(2)]
            dg = [sb("dg%d" % i, [128, CONV_K, 128], BF16) for i in range(2)]
            sig = [sb("sig%d" % i, [128, 512], F32) for i in range(2)]
            spA = sb("spA", [128, 36 + 12 * CONV_K], F32)
            stt = [sb("stt%d" % i, [128, 512], F32) for i in range(4)]
            sq = [sb("sq%d" % i, [128, 512], BF16) for i in range(2)]
            scr = {"id": "A", "p": [sb("pA%d" % i, [128, 256], BF16) for i in range(2)],
                   "pT": sb("pTA", [128, 2, 128], BF16), "sm": self.sm}
            ytok = sb("ytokA", [128, 512], BF16)

            self.DMA(spA[:], self.a_sp[j], "spA", r=[], w=["spA"])
            for i in range(2):
                self.MEMSET("dve", hbuf[i][:, 0:32], 0.0, w=[("h", i)])
            self.mem_kv(self.a_w_mkv[j])

            for g in range(2):
                pieces, n = self.win_pieces(w_in, [(3072 + g * 256, 256)])
                wv, wk = self.wload(pieces, [128, 8, 256])
                for ct in range(2):
                    f = 12 + g * 2 + ct
                    for tb in range(4):
                        def ev(pi, pk, f=f, tb=tb):
                            self.CP("act", self.QY[:, f, tb * 512:(tb + 1) * 512], self.PS[pi][:, :], r=[pk],
                                    w=[("QY", f, tb * 4 + q) for q in range(4)])
                        self.proj_fm(wv, wk, ct, tb, ev)

            mgen = self.mem_attention(scr, ytok)
            for c in range(12):
                hs = c % 2
                hk = ("h", hs)
                pieces, n = self.win_pieces(w_in, [(c * 128, 128), (1536 + c * 128, 128)])
                wv, wk = self.wload(pieces, [128, 8, 256])
                self.TT("dve", dg[hs][:, :, :],
                        self.identf[:, :].unsqueeze(1).to_broadcast([128, CONV_K, 128]),
                        spA[:, 36 + c * CONV_K:36 + (c + 1) * CONV_K].unsqueeze(2).to_broadcast([128, CONV_K, 128]),
                        ALU.mult, r=["identf", "spA"], w=[("dg", hs)])
                for tb in range(4):
                    ss = tb % 2
                    def ev_g(pi, pk, ss=ss):
                        self.ACT(sig[ss][:, :], self.PS[pi][:, :], AF.Sigmoid, r=[pk], w=[("sig", ss)])
                    def ev_a(pi, pk, ss=ss, tb=tb, hs=hs, hk=hk):
                        self.TT("dve", hbuf[hs][:, 30 + tb * 512:30 + (tb + 1) * 512], self.PS[pi][:, :], sig[ss][:, :],
                                ALU.mult, r=[pk, ("sig", ss)], w=[(hk, tb)])
                    self.proj_fm(wv, wk, 1, tb, ev_g)
                    self.proj_fm(wv, wk, 0, tb, ev_a)
                for tb in range(4):
                    pi = self.psum(); pk = ("ps", pi)
                    rk = [hk, ("dg", hs)] + [(hk, t2) for t2 in range(max(0, tb - 1), tb + 1)]
                    for k in range(CONV_K):
                        self.MM(self.PS[pi][:, :], dg[hs][:, k, :], hbuf[hs][:, tb * 512 + k:tb * 512 + k + 512],
                                k == 0, k == CONV_K - 1, r=rk, w=[pk])
                    self.ACT(self.QY[:, c, tb * 512:(tb + 1) * 512], self.PS[pi][:, :], AF.Identity, r=[pk, "spA"],
                             w=[("QY", c, tb * 4 + q) for q in range(4)], bias=spA[:, c:c + 1], scale=1.0)
            for _ in mgen:
                pass

            for tb in range(4):
                tsl = slice(tb * 512, (tb + 1) * 512)
                p1 = self.psum(); p2 = self.psum()
                for c in range(12):
                    qk = [("QY", c, tb * 4 + q) for q in range(4)]
                    ss = c % 2
                    self.ACT(sq[ss][:, :], self.QY[:, c, tsl], AF.Square, r=qk, w=[("sq", ss)])
                    self.MM(self.PS[p1][:, :], self.ones[:, :], self.QY[:, c, tsl], c == 0, c == 11, r=qk + ["ones"], w=[("ps", p1)])
                    self.MM(self.PS[p2][:, :], self.ones[:, :], sq[ss][:, :], c == 0, c == 11, r=[("sq", ss), "ones"], w=[("ps", p2)])
                mean, msq, var, rstd = stt
                self.ACT(mean[:, :], self.PS[p1][:, :], AF.Identity, r=[("ps", p1)], w=["stt0"], scale=1.0 / CONV_W)
                self.TT("dve", msq[:, :], mean[:, :], mean[:, :], ALU.mult, r=["stt0"], w=["stt1"])
                self.STT("dve", var[:, :], self.PS[p2][:, :], 1.0 / CONV_W, msq[:, :], ALU.mult, ALU.subtract,
                         r=[("ps", p2), "stt1"], w=["stt2"])
                self.TS("dve", var[:, :], var[:, :], LN_EPS, None, ALU.add, None, r=["stt2"], w=["stt2"])
                self.S.op("act", lambda e, var=var: e.sqrt(var[:, :], var[:, :]), r=["stt2"], w=["stt2"])
                self.S.op("dve", lambda e, var=var, rstd=rstd: e.reciprocal(rstd[:, :], var[:, :]), r=["stt2"], w=["stt3"])
                for c in range(12):
                    qk = [("QY", c, tb * 4 + q) for q in range(4)]
                    ss = c % 2
                    self.TT("dve", sig[ss][:, :], self.QY[:, c, tsl], mean[:, :], ALU.subtract, r=qk + ["stt0"], w=[("sig", ss)])
                    self.TT("dve", sig[ss][:, :], sig[ss][:, :], rstd[:, :], ALU.mult, r=[("sig", ss), "stt3"], w=[("sig", ss)])
                    self.ACT(self.QY[:, c, tsl], sig[ss][:, :], AF.Silu, r=[("sig", ss), "spA"], w=qk,
                             bias=spA[:, 24 + c:25 + c], scale=spA[:, 12 + c:13 + c])

    def layer_B(self, j):
        nc = self.nc
        S = self.S
        w_in = self.b_w_in[j]
        slopes = _alibi_slopes(ATT_H)
        with ExitStack() as st:
            sb = lambda name, shape, dtype: st.enter_context(nc.sbuf_tensor(self.nm(name), shape, dtype))
            kT = sb("kT", [128, SEQ], BF16)
            v = sb("v", [128, 16, 128], BF16)
            qiT = sb("qiT", [128, 4, SEQ], BF16)
            kiT2 = sb("kiT2", [128, SEQ], BF16)
            wi = sb("wi", [128, 16, 8], F32)
            dtab = sb("dtab", [128, SEQ], F32)
            madm = sb("madm", [128, 256], F32)
            zz = [sb("zB%d" % i, [128, SEQ], F32) for i in range(2)]
            z = zz[0]
            M = sb("MB", [128, SEQ], BF16)
            pp = [sb("pB%d" % i, [128, SEQ], BF16) for i in range(2)]
            junk = pp[0]
            pT = sb("pTB", [128, 16, 128], BF16)
            ytok = sb("ytokB", [128, CONV_W], BF16)
            rl = [sb("rl%d" % i, [128, 512], F32) for i in range(2)]
            sm = self.sm
            scr = {"id": "B", "z": zz, "p": pp, "pT": pT, "sm": sm}
            zk = ("z", ("B", 0))
            jk = ("p", ("B", 0))

            self.DMA(dtab[:], self.c_dtab[:, :], "cst", r=[], w=["dtab"])
            self.DMA(madm[:], self.c_madm[:, :], "cst", r=[], w=["madm"])

            def fm_group(cols, dests, side=None):
                pieces, n = self.win_pieces(w_in, cols)
                wv, wk = self.wload(pieces, [128, 8, n])
                for ct, (dst_fn, key_fn) in enumerate(dests):
                    for tb in range(4):
                        def ev(pi, pk, dst_fn=dst_fn, key_fn=key_fn, tb=tb):
                            self.CP("act", dst_fn(tb), self.PS[pi][:, :], r=[pk], w=key_fn(tb))
                        self.proj_fm(wv, wk, ct, tb, ev)
                        if side is not None:
                            next(side, None)
                            next(side, None)

            def qy_dst(f):
                return ((lambda tb: self.QY[:, f, tb * 512:(tb + 1) * 512]),
                        (lambda tb: [("QY", f, tb * 4 + q) for q in range(4)]))

            fm_group([(1536, 128), (2304, 64), (2304, 64)],
                     [((lambda tb: kT[:, tb * 512:(tb + 1) * 512]), (lambda tb: [("kT", tb)])),
                      ((lambda tb: kiT2[:, tb * 512:(tb + 1) * 512]), (lambda tb: [("kiT2", tb)]))])
            pieces, n = self.win_pieces(w_in, [(1664, 128), (2368, 8)])
            wv, wk = self.wload(pieces, [128, 8, 136])
            for t in range(16):
                pi = self.psum(); pk = ("ps", pi)
                for k in range(8):
                    self.MM(self.PS[pi][:, 0:136], self.xT[:, k, t * 128:(t + 1) * 128], wv[:, k, :], k == 0, k == 7,
                            r=[wk, ("xT", t // 4)], w=[pk])
                self.CP("act", v[:, t, :], self.PS[pi][:, 0:128], r=[pk], w=[("v", t)])
                self.CP("act", wi[:, t, :], self.PS[pi][:, 128:136], r=[pk], w=[("wi", t)])
            for g in range(2):
                fm_group([(1792 + g * 256, 256)],
                         [((lambda tb, f=g * 2 + c: qiT[:, f, tb * 512:(tb + 1) * 512]),
                           (lambda tb, f=g * 2 + c: [("qiT", f, tb)])) for c in range(2)])
            for g in range(2):
                fm_group([(2376 + g * 256, 256)], [qy_dst(12 + g * 2), qy_dst(12 + g * 2 + 1)])
            self.mem_kv(self.b_w_mkv[j])
            mgen = self.mem_attention(scr, ytok)
            for g in range(6):
                fm_group([(g * 256, 256)], [qy_dst(g * 2), qy_dst(g * 2 + 1)])
            for _ in mgen:
                pass

            cA, cLO, cW, cMID, cCNT, cG = 8, 9, 10, 11, 12, 13
            col = lambda c: sm[:, c:c + 1]
            ck = lambda c: ("smb", c)
            isc = self.wst[1]
            isck = ("wst", 1)
            junk = self.wbf[0]
            jk = ("wbf", 0)
            Mb = [M, self.wbf[1]]
            Mk = [[("M", 0)], [("M", 1), ("wbf", 1)]]

            def mask_gen(qt):
                nk = 128 * (qt + 1)
                nch = (nk + 511) // 512
                Mq = Mb[qt % 2]
                mk = Mk[qt % 2]
                if qt < 2:
                    if nk > 128:
                        self.MEMSET("dve", Mq[:, 0:nk - 128], 0.0, w=mk)
                    self.CP("dve", Mq[:, nk - 128:nk], madm[:, 0:128], r=["madm"], w=mk)
                    yield
                    return
                for h in range(IDX_H):
                    hb = (h % 2) * 64
                    for c in range(nch):
                        c0 = c * 512
                        n = min(512, nk - c0)
                        pi = self.psum(); pk = ("ps", pi)
                        ss = (h * nch + c) % 2
                        self.MM(self.PS[pi][:, 0:n], qiT[hb:hb + 64, h // 2, qt * 128:(qt + 1) * 128], kiT2[hb:hb + 64, c0:c0 + n],
                                True, True, r=[("qiT", h // 2, qt // 4), ("kiT2", c)], w=[pk])
                        self.ACT(rl[ss][:, 0:n], self.PS[pi][:, 0:n], AF.Relu, r=[pk], w=[("rl", ss)])
                        if h == 0:
                            self.TS("dve", isc[:, c0:c0 + n], rl[ss][:, 0:n], wi[:, qt, 0:1], None, ALU.mult, None,
                                    r=[("rl", ss), ("wi", qt)], w=[isck])
                        else:
                            self.STT("dve", isc[:, c0:c0 + n], rl[ss][:, 0:n], wi[:, qt, h:h + 1], isc[:, c0:c0 + n], ALU.mult, ALU.add,
                                     r=[("rl", ss), ("wi", qt), isck], w=[isck])
                        yield
                self.TS("dve", junk[:, 0:nk], isc[:, 0:nk], 0.0, None, ALU.add, ALU.max, r=[isck], w=[jk, ck(cA)], accum=col(cA))
                self.TS("dve", junk[:, 0:nk], isc[:, 0:nk], 0.0, None, ALU.add, ALU.min, r=[isck], w=[jk, ck(cLO)], accum=col(cLO))
                yield
                self.TT("dve", col(cW), col(cA), col(cLO), ALU.subtract, r=[ck(cA), ck(cLO)], w=[ck(cW)])
                self.TS("dve", col(cW), col(cW), 1.000002, 1e-20, ALU.mult, ALU.add, r=[ck(cW)], w=[ck(cW)])
                self.TT("dve", isc[:, nk - 128:nk], isc[:, nk - 128:nk], madm[:, 128:256], ALU.add, r=[isck, "madm"], w=[isck])
                yield
                for it in range(NBISECT):
                    cn = 2.0 ** (-(it + 1))
                    self.STT("dve", col(cMID), col(cW), cn, col(cLO), ALU.mult, ALU.add, r=[ck(cW), ck(cLO)], w=[ck(cMID)])
                    self.TS("dve", junk[:, 0:nk], isc[:, 0:nk], col(cMID), None, ALU.is_ge, ALU.add, r=[isck, ck(cMID)],
                            w=[jk, ck(cCNT)], accum=col(cCNT))
                    self.TS("dve", col(cG), col(cCNT), float(TOPK) - 0.5, cn, ALU.is_ge, ALU.mult, r=[ck(cCNT)], w=[ck(cG)])
                    self.STT("dve", col(cLO), col(cG), col(cW), col(cLO), ALU.mult, ALU.add, r=[ck(cG), ck(cW), ck(cLO)], w=[ck(cLO)])
                    yield
                self.TS("dve", Mq[:, 0:nk], isc[:, 0:nk], col(cLO), MASKNEG, ALU.is_lt, ALU.mult, r=[isck, ck(cLO)], w=mk)
                yield

            for _ in mask_gen(0):
                pass
            for qt in range(16):
                nk = 128 * (qt + 1)
                nch = (nk + 511) // 512
                d0 = 1920 - 128 * qt
                jobs = []
                for h in range(ATT_H):
                    jobs.append(dict(qT=self.QY[:, h, qt * 128:(qt + 1) * 128], qkeys=[("QY", h, qt)],
                                     kT=kT[:, 0:nk], kkeys=[("kT", c) for c in range(nch)], nk=nk,
                                     vfn=(lambda c: v[:, c, :]), vkeys=[("v", c) for c in range(nk // 128)],
                                     bias=(dtab[:, d0:d0 + nk], -slopes[h] / SCALE, Mb[qt % 2], [("M", qt % 2)]),
                                     ydst=ytok[:, h * 128:(h + 1) * 128], ydkey=("ytok", h), post=None))
                side = mask_gen(qt + 1) if qt < 15 else None
                self.attn_pipeline(jobs, scr, side=side, per=5)
                if side is not None:
                    for _ in side:
                        pass
                self.ytok_to_QY(ytok, [("ytok", h) for h in range(ATT_H)], 0, ATT_H, qt)

    def gate_out(self, wgate, goff, wout, pgbsrc, xsrc, xdst, last, li):
        nc = self.nc
        S = self.S
        with ExitStack() as st:
            sb = lambda name, shape, dtype: st.enter_context(nc.sbuf_tensor(self.nm(name), shape, dtype))
            woutb = sb("woutb", [128, 16, D_MODEL], BF16)
            self.xio = [sb("xio%d" % i, [128, D_MODEL], F32) for i in range(2)]
            self.xb = [sb("xb%d" % i, [128, D_MODEL], BF16) for i in range(2)]
            self.pgb = sb("pgb", [128, 2 * D_MODEL], F32)
            self.lnst = sb("lnst", [128, 12], F32)
            self.lnag = sb("lnag", [128, 4], F32)
            gt = [sb("gt%d" % i, [128, 512], BF16) for i in range(2)]
            self.DMA(self.pgb[:], pgbsrc, "pgb", r=[], w=["pgb"])
            for g in range(8):
                pieces, n = self.win_pieces(wgate, [(goff + g * 256, 256)])
                wv, wk = self.wload(pieces, [128, 8, 256])
                for ct in range(2):
                    f = g * 2 + ct
                    for tb in range(4):
                        def ev(pi, pk, f=f, tb=tb):
                            ss = tb % 2
                            qk = [("QY", f, tb * 4 + q) for q in range(4)]
                            self.ACT(gt[ss][:, :], self.PS[pi][:, :], AF.Silu, r=[pk], w=[("gt", ss)])
                            self.TT("dve", self.QY[:, f, tb * 512:(tb + 1) * 512], self.QY[:, f, tb * 512:(tb + 1) * 512],
                                    gt[ss][:, :], ALU.mult, r=qk + [("gt", ss)], w=qk)
                        self.proj_fm(wv, wk, ct, tb, ev)
                s = self.w_rr % 2
                self.w_rr += 1
                stv = self.wst[s][:, :].rearrange("p (a b) -> p a b", a=2)
                self.DMA(stv, wout[g * 256:(g + 1) * 256, :].rearrange("(k p) c -> p k c", p=128), "wst%d" % s, r=[], w=[("wst", s)])
                self.CP("act", woutb[:, 2 * g:2 * g + 2, :], stv, r=[("wst", s)], w=[("wout", g)])
            for t in range(16):
                s = t % 2
                xk = ("xio", s)
                self.DMA(self.xio[s][:], xsrc[t * 128:(t + 1) * 128, :], "xio%d" % s, r=[("xdram", li - 1, t)], w=[xk])
                for half in range(2):
                    pi = self.psum(); pk = ("ps", pi)
                    for f in range(16):
                        self.MM(self.PS[pi][:, :], self.QY[:, f, t * 128:(t + 1) * 128], woutb[:, f, half * 512:(half + 1) * 512],
                                f == 0, f == 15, r=[("QY", f, t), ("wout", f // 2)], w=[pk])
                    self.STT("dve", self.xio[s][:, half * 512:(half + 1) * 512], self.xio[s][:, half * 512:(half + 1) * 512], ALPHA,
                             self.PS[pi][:, :], ALU.mult, ALU.add, r=[pk, xk], w=[xk])
                self.layernorm_tile(s)
                self.DMA(xdst[t * 128:(t + 1) * 128, :], self.xio[s][:], "xout%d" % s, r=[xk], w=[("xdram", li, t)])
                if not last:
                    self.to_T(s, self.xT, t, ("xT", t // 4))


_PROG_CACHE = {}


def _get_prog(layers):
    key = tuple(layers)
    if key not in _PROG_CACHE:
        p = Prog(layers)
        p.build()
        _PROG_CACHE[key] = p
    return _PROG_CACHE[key]


def _consts():
    ident = np.eye(128, dtype=np.float32)
    p = np.arange(128, dtype=np.float32)[:, None]
    jj = np.arange(SEQ, dtype=np.float32)[None, :]
    dtab = np.abs(p - (jj - 1920.0)).astype(np.float32)
    madm = np.zeros((128, 256), dtype=np.float32)
    madm[0:64, 64:128] = MASKNEG
    madm[0:64, 128 + 64:256] = -1e30
    return ident, dtab, madm


def _host_inputs(inp, b):
    f = lambda a: np.ascontiguousarray(np.asarray(a, dtype=np.float32))
    ident, dtab, madm = _consts()
    rep = lambda v: np.ascontiguousarray(np.broadcast_to(np.asarray(v, np.float32)[None, :], (128, v.shape[-1])))
    a_sp = np.zeros((2, 128, 36 + 12 * CONV_K), np.float32)
    for j in range(2):
        a_sp[j, :, 0:12] = np.asarray(inp["a_conv_b"][j]).reshape(12, 128).T
        a_sp[j, :, 12:24] = np.asarray(inp["a_ln_g"][j]).reshape(12, 128).T
        a_sp[j, :, 24:36] = np.asarray(inp["a_ln_b"][j]).reshape(12, 128).T
        cw = np.asarray(inp["a_conv_w"][j]).reshape(CONV_K, 12, 128)
        a_sp[j, :, 36:] = np.transpose(cw, (2, 1, 0)).reshape(128, 12 * CONV_K)
    a_pgb = np.stack([np.concatenate([rep(inp["a_post_g"][j]), rep(inp["a_post_b"][j])], axis=1) for j in range(2)])
    b_pgb = np.stack([np.concatenate([rep(inp["b_post_g"][j]), rep(inp["b_post_b"][j])], axis=1) for j in range(2)])
    memgb = np.concatenate([rep(np.asarray(inp["mem_ln_g"])), rep(np.asarray(inp["mem_ln_b"]))], axis=1)
    return {
        "x": f(inp["x"][b]), "mem": f(inp["mem"][b]), "memgb": f(memgb),
        "c_ident": ident, "c_dtab": dtab, "c_madm": madm,
        "a_w_in": f(inp["a_w_in"]), "a_sp": f(a_sp), "a_w_mkv": f(inp["a_w_mkv"]), "a_w_out": f(inp["a_w_out"]),
        "a_pgb": f(a_pgb),
        "b_w_in": f(inp["b_w_in"]), "b_w_mkv": f(inp["b_w_mkv"]), "b_w_out": f(inp["b_w_out"]), "b_pgb": f(b_pgb),
    }


def run_layers(inp, layers, cores):
    prog = _get_prog(layers)
    maps = [_host_inputs(inp, b) for b in cores]
    shared = {}
    for m in maps[1:]:
        for k in m:
            if k not in ("x", "mem"):
                m[k] = maps[0][k]
    res = run_bass_kernel_spmd(prog.nc, maps, core_ids=list(range(len(cores))))
    if DEBUG:
        global _DBG
        _DBG = [{k: np.asarray(r[k]) for k in ("dbg", "dbg_k", "dbg_v", "dbg_m")} for r in res.results]
    return np.stack([r["out"] for r in res.results], axis=0)


def kernel(**inputs):
    inp = {k: np.asarray(v) for k, v in inputs.items()}
    out = run_layers(inp, [0, 1, 2, 3], list(range(8)))
    return out.astype(np.float32)
```

```python
import numpy as np
from contextlib import ExitStack
import concourse.bass as bass
import concourse.mybir as mybir
from concourse.bass_utils import run_bass_kernel_spmd

F32 = mybir.dt.float32
BF16 = mybir.dt.bfloat16
AF = mybir.ActivationFunctionType
ALU = mybir.AluOpType
AX = mybir.AxisListType

D_MODEL = 1024
SEQ = 2048
DEPTH = 4
N_MEM = 256
HD = 128
CONV_W = 1536
CONV_K = 31
ATT_H = 12
IDX_H = 8
IDX_D = 64
TOPK = 256
ALPHA = (2 * DEPTH) ** 0.25
LN_EPS = 1e-5
SCALE = HD ** -0.5
A_IN = 5632
B_IN = 4936
NBISECT = 20
DEBUG = False
MASKNEG = -30000.0


def _alibi_slopes(n):
    import math
    p = 2 ** int(math.floor(math.log2(n)))
    base = [2.0 ** (-8.0 * (i + 1) / p) for i in range(p)]
    extra = [2.0 ** (-4.0 * (2 * i + 1) / p) for i in range(n - p)]
    return base + extra


class _Op:
    __slots__ = ("eng", "fn", "deps", "dma", "sig", "cnt", "waits", "clock", "gid")


class Sched:
    ENG = ("pe", "act", "dve", "pool", "sp")

    def __init__(self):
        self.ops = []
        self.lastw = {}
        self.readers = {}
        self.streams = {}
        self.last_on = {}
        self.pending_dma = []

    def op(self, eng, fn, r=(), w=(), dma=None, extra=()):
        o = _Op()
        o.eng = eng; o.fn = fn; o.dma = dma; o.sig = False; o.gid = len(self.ops)
        o.cnt = 0; o.clock = None; o.waits = ()
        deps = {}

        def add(d, kind):
            if d is None or d is o:
                return
            if d.dma is None and dma is None and d.eng == eng:
                if eng == "pe":
                    return
                if kind == "war":
                    return
            deps[d.gid] = d

        for k in r:
            add(self.lastw.get(k), "raw")
            if isinstance(k, tuple) and k[0] == "ps":
                rd = self.readers.get(k)
                if rd:
                    for x in rd.values():
                        if x.eng != eng:
                            add(x, "raw")
        for k in w:
            add(self.lastw.get(k), "waw")
            rd = self.readers.get(k)
            if rd:
                for x in rd.values():
                    add(x, "war")
        for d in extra:
            add(d, "raw")
        for k in w:
            self.lastw[k] = o
            self.readers[k] = {}
        for k in r:
            rd = self.readers.setdefault(k, {})
            rd[("dma", o.gid) if dma is not None else eng] = o
        o.deps = list(deps.values())
        self.ops.append(o)
        self.last_on[eng] = o
        if dma is not None:
            self.streams[dma] = self.streams.get(dma, 0) + 1
            o.cnt = self.streams[dma] * 16
            self.pending_dma.append(o)
        return o

    def barrier(self):
        last = [self.last_on[e] for e in self.ENG if e in self.last_on]
        dmas = list(self.pending_dma)
        self.pending_dma = []
        for e in self.ENG:
            ex = [d for d in last if d.eng != e or d.dma is not None] + dmas
            self.op(e, (lambda en: en.nop()), extra=ex)

    def finalize(self):
        for o in self.ops:
            for d in o.deps:
                d.sig = True
        cnt = {e: 0 for e in self.ENG}
        for o in self.ops:
            if o.dma is None and o.sig:
                cnt[o.eng] += 1
                o.cnt = cnt[o.eng]
        eclock = {e: {} for e in self.ENG}
        for o in self.ops:
            ck = eclock[o.eng]
            waits = {}
            for d in sorted(o.deps, key=lambda t: t.gid):
                key = d.dma if d.dma is not None else d.eng
                if ck.get(key, 0) >= d.cnt:
                    continue
                waits[key] = max(waits.get(key, 0), d.cnt)
                for k2, v2 in d.clock.items():
                    if ck.get(k2, 0) < v2:
                        ck[k2] = v2
            o.waits = tuple(waits.items())
            if o.dma is not None:
                c2 = dict(ck); c2[o.dma] = max(c2.get(o.dma, 0), o.cnt); o.clock = c2
            elif o.sig:
                c2 = dict(ck); c2[o.eng] = o.cnt; o.clock = c2
        self.byeng = {e: [o for o in self.ops if o.eng == e] for e in self.ENG}

    def emit(self, nc, stack):
        self.finalize()
        sems = {}
        for key in list(self.ENG) + list(self.streams.keys()):
            sems[key] = stack.enter_context(nc.semaphore("s_" + str(key)))
        streams = self.streams
        byeng = self.byeng

        def runner(engname):
            def f(e):
                for o in byeng[engname]:
                    for key, val in o.waits:
                        e.wait_ge(sems[key], val)
                    ins = o.fn(e)
                    if o.dma is not None:
                        ins.then_inc(sems[o.dma], 16)
                    elif o.sig:
                        ins.then_inc(sems[o.eng], 1)
                if engname == "sp":
                    for key, n in streams.items():
                        e.wait_ge(sems[key], 16 * n)
            return f

        with nc.Block() as block:
            block.tensor(runner("pe"))
            block.scalar(runner("act"))
            block.vector(runner("dve"))
            block.gpsimd(runner("pool"))
            block.sync(runner("sp"))


class Prog:
    def __init__(self, layers, final_to_out=True):
        self.layers = list(layers)
        self.S = Sched()
        self.nc = bass.Bass("TRN2", target_bir_lowering=False)
        self.ps_rr = 0
        self.uid = 0

    def nm(self, name):
        self.ncnt = getattr(self, "ncnt", 0) + 1
        return "%s_%d" % (name, self.ncnt)

    def MM(self, out, lhsT, rhs, start, stop, r, w):
        return self.S.op("pe", lambda e: e.matmul(out, lhsT, rhs, start=start, stop=stop), r=r, w=w)

    def TR(self, out, in_, ident, r, w):
        return self.S.op("pe", lambda e: e.transpose(out, in_, ident), r=r, w=w)

    def ACT(self, out, in_, func, r, w, bias=None, scale=None, accum=None):
        kw = {}
        if bias is not None:
            kw["bias"] = bias
        if scale is not None:
            kw["scale"] = scale
        if accum is not None:
            kw["accum_out"] = accum
        return self.S.op("act", lambda e: e.activation(out=out, in_=in_, func=func, **kw), r=r, w=w)

    def TS(self, eng, out, in0, s1, s2, op0, op1, r, w, accum=None):
        kw = {}
        if op1 is not None:
            kw["op1"] = op1
        if accum is not None:
            kw["accum_out"] = accum
        return self.S.op(eng, lambda e: e.tensor_scalar(out=out, in0=in0, scalar1=s1, scalar2=s2, op0=op0, **kw), r=r, w=w)

    def TT(self, eng, out, in0, in1, op, r, w):
        return self.S.op(eng, lambda e: e.tensor_tensor(out=out, in0=in0, in1=in1, op=op), r=r, w=w)

    def STT(self, eng, out, in0, scalar, in1, op0, op1, r, w):
        return self.S.op(eng, lambda e: e.scalar_tensor_tensor(out=out, in0=in0, scalar=scalar, in1=in1, op0=op0, op1=op1), r=r, w=w)

    def CP(self, eng, out, in_, r, w):
        if eng == "act":
            return self.S.op("act", lambda e: e.copy(out=out, in_=in_), r=r, w=w)
        return self.S.op(eng, lambda e: e.tensor_copy(out=out, in_=in_), r=r, w=w)

    def MEMSET(self, eng, ap, val, w):
        return self.S.op(eng, lambda e: e.memset(ap, val), w=w)

    def DMA(self, out, in_, stream, r, w):
        return self.S.op("sp", lambda e: e.dma_start(out=out, in_=in_), r=r, w=w, dma=stream)

    def psum(self):
        i = self.ps_rr
        self.ps_rr = (self.ps_rr + 1) % 8
        return i

    def wload(self, pieces, shape):
        s = self.w_rr % 2
        b = self.wb_rr % 2
        self.w_rr += 1
        self.wb_rr += 1
        a, bb = shape[1], shape[2]
        n = a * bb
        assert n <= 2048
        stv = self.wst[s][:, 0:n].rearrange("p (a b) -> p a b", a=a)
        bfv = self.wbf[b][:, 0:n].rearrange("p (a b) -> p a b", a=a)
        for dst_fn, src in pieces:
            self.DMA(dst_fn(stv), src, "wst%d" % s, r=[], w=[("wst", s)])
        self.CP("pool", bfv, stv, r=[("wst", s)], w=[("wbf", b)])
        return bfv, ("wbf", b)

    def build(self):
        nc = self.nc
        S = self.S
        L = self.layers
        dt = nc.dram_tensor
        self.x_in = dt("x", [SEQ, D_MODEL], F32, kind="ExternalInput").ap()
        self.mem_in = dt("mem", [N_MEM, D_MODEL], F32, kind="ExternalInput").ap()
        self.memgb = dt("memgb", [128, 2 * D_MODEL], F32, kind="ExternalInput").ap()
        self.c_ident = dt("c_ident", [128, 128], F32, kind="ExternalInput").ap()
        self.c_dtab = dt("c_dtab", [128, SEQ], F32, kind="ExternalInput").ap()
        self.c_madm = dt("c_madm", [128, 256], F32, kind="ExternalInput").ap()
        self.a_w_in = dt("a_w_in", [2, D_MODEL, A_IN], F32, kind="ExternalInput").ap()
        self.a_sp = dt("a_sp", [2, 128, 36 + 12 * CONV_K], F32, kind="ExternalInput").ap()
        self.a_w_mkv = dt("a_w_mkv", [2, D_MODEL, 1024], F32, kind="ExternalInput").ap()
        self.a_w_out = dt("a_w_out", [2, 2048, D_MODEL], F32, kind="ExternalInput").ap()
        self.a_pgb = dt("a_pgb", [2, 128, 2 * D_MODEL], F32, kind="ExternalInput").ap()
        self.b_w_in = dt("b_w_in", [2, D_MODEL, B_IN], F32, kind="ExternalInput").ap()
        self.b_w_mkv = dt("b_w_mkv", [2, D_MODEL, 1024], F32, kind="ExternalInput").ap()
        self.b_w_out = dt("b_w_out", [2, 2048, D_MODEL], F32, kind="ExternalInput").ap()
        self.b_pgb = dt("b_pgb", [2, 128, 2 * D_MODEL], F32, kind="ExternalInput").ap()
        self.out = dt("out", [SEQ, D_MODEL], F32, kind="ExternalOutput").ap()
        self.dbg = dt("dbg", [128, 16 * SEQ], BF16, kind="ExternalOutput").ap() if DEBUG else None
        if DEBUG:
            self.dbg_k = dt("dbg_k", [128, 4 * 256], BF16, kind="ExternalOutput").ap()
            self.dbg_v = dt("dbg_v", [128, 2 * 512], BF16, kind="ExternalOutput").ap()
            self.dbg_m = dt("dbg_m", [128, 8 * 256], BF16, kind="ExternalOutput").ap()
        self.xs = [dt("xs0", [SEQ, D_MODEL], F32).ap(), dt("xs1", [SEQ, D_MODEL], F32).ap()]

        with ExitStack() as st:
            sb = lambda name, shape, dtype: st.enter_context(nc.sbuf_tensor(self.nm(name), shape, dtype))
            self.PS = [st.enter_context(nc.psum_tensor("ps%d" % i, [128, 512], F32)) for i in range(8)]
            self.identf = sb("identf", [128, 128], F32)
            self.ident = sb("ident", [128, 128], BF16)
            self.ones = sb("ones", [128, 128], BF16)
            self.memT = sb("memT", [128, 8, N_MEM], BF16)
            self.xT = sb("xT", [128, 8, SEQ], BF16)
            self.QY = sb("QY", [128, 16, SEQ], BF16)
            self.wst = [sb("wst%d" % i, [128, 2048], F32) for i in range(2)]
            self.wbf = [sb("wbf%d" % i, [128, 2048], BF16) for i in range(2)]
            self.kTm = sb("kTm", [128, 4, N_MEM], BF16)
            self.vm = sb("vm", [128, 2, 512], BF16)
            self.sm = sb("sm", [128, 64], F32)
            self.w_rr = 0
            self.wb_rr = 0

            self.DMA(self.identf[:], self.c_ident[:, :], "cst", r=[], w=["identf"])
            self.CP("dve", self.ident[:], self.identf[:], r=["identf"], w=["ident"])
            self.MEMSET("dve", self.ones[:], 1.0, w=["ones"])

            with ExitStack() as st0:
                sb0 = lambda name, shape, dtype: st0.enter_context(nc.sbuf_tensor(self.nm(name), shape, dtype))
                self.xio = [sb0("xio%d" % i, [128, D_MODEL], F32) for i in range(2)]
                self.xb = [sb0("xb%d" % i, [128, D_MODEL], BF16) for i in range(2)]
                self.pgb = sb0("pgb", [128, 2 * D_MODEL], F32)
                self.lnst = sb0("lnst", [128, 12], F32)
                self.lnag = sb0("lnag", [128, 4], F32)
                self.DMA(self.pgb[:], self.memgb[:, :], "pgb", r=[], w=["pgb"])
                for t in range(2):
                    s = t % 2
                    self.DMA(self.xio[s][:], self.mem_in[t * 128:(t + 1) * 128, :], "xio%d" % s, r=[], w=[("xio", s)])
                    self.layernorm_tile(s)
                    self.to_T(s, self.memT, t, ("memT", t))
                for t in range(16):
                    s = t % 2
                    self.DMA(self.xio[s][:], self.x_in[t * 128:(t + 1) * 128, :], "xio%d" % s, r=[], w=[("xio", s)])
                    self.CP("act", self.xb[s][:], self.xio[s][:], r=[("xio", s)], w=[("xb", s)])
                    self.to_T(s, self.xT, t, ("xT", t // 4))
            S.barrier()

            xsrc = self.x_in
            for li, layer in enumerate(L):
                j = layer // 2
                last = (li == len(L) - 1)
                xdst = self.out if last else self.xs[li % 2]
                if layer % 2 == 0:
                    self.layer_A(j)
                    wout, pgbsrc, wgate, goff = self.a_w_out[j], self.a_pgb[j], self.a_w_in[j], 3584
                else:
                    self.layer_B(j)
                    wout, pgbsrc, wgate, goff = self.b_w_out[j], self.b_pgb[j], self.b_w_in[j], 2888
                S.barrier()
                if DEBUG and li == 0:
                    self.DMA(self.dbg[:, :], self.QY[:, :, :].rearrange("p f t -> p (f t)"), "dbg", r=[("QY", f, t) for f in range(16) for t in range(16)], w=[])
                    self.DMA(self.dbg_k[:, :], self.kTm[:, :, :].rearrange("p f t -> p (f t)"), "dbg", r=[("kTm", h) for h in range(4)], w=[])
                    self.DMA(self.dbg_v[:, :], self.vm[:, :, :].rearrange("p f t -> p (f t)"), "dbg", r=[("vm", 0, 0)], w=[])
                    self.DMA(self.dbg_m[:, :], self.memT[:, :, :].rearrange("p f t -> p (f t)"), "dbg", r=[("memT", 0)], w=[])
                    S.barrier()
                self.gate_out(wgate, goff, wout, pgbsrc, xsrc, xdst, last, li)
                S.barrier()
                xsrc = xdst
            S.emit(nc, st)
        return nc

    def layernorm_tile(self, s):
        xk = ("xio", s)
        x = self.xio[s]
        lnst, lnag, pgb, xb = self.lnst, self.lnag, self.pgb, self.xb[s]
        S = self.S
        for c in range(2):
            S.op("dve", (lambda e, c=c, lnst=lnst, x=x: e.bn_stats(lnst[:, c * 6:(c + 1) * 6], x[:, c * 512:(c + 1) * 512])),
                 r=[xk], w=[("lnst", c)])
        S.op("dve", (lambda e, lnst=lnst, lnag=lnag: e.bn_aggr(lnag[:, 0:2], lnst[:, :])), r=[("lnst", 0), ("lnst", 1)], w=["lnag"])
        self.TS("dve", lnag[:, 2:3], lnag[:, 1:2], LN_EPS, None, ALU.add, None, r=["lnag"], w=["lnrs0"])
        S.op("act", (lambda e, lnag=lnag: e.sqrt(lnag[:, 2:3], lnag[:, 2:3])), r=["lnrs0"], w=["lnrs0"])
        S.op("dve", (lambda e, lnag=lnag: e.reciprocal(lnag[:, 3:4], lnag[:, 2:3])), r=["lnrs0"], w=["lnrs"])
        self.TS("dve", x[:], x[:], lnag[:, 0:1], lnag[:, 3:4], ALU.subtract, ALU.mult, r=[xk, "lnag", "lnrs"], w=[xk])
        self.TT("dve", x[:], x[:], pgb[:, 0:D_MODEL], ALU.mult, r=[xk, "pgb"], w=[xk])
        self.TT("dve", x[:], x[:], pgb[:, D_MODEL:2 * D_MODEL], ALU.add, r=[xk, "pgb"], w=[xk])
        self.CP("act", xb[:], x[:], r=[xk], w=[("xb", s)])

    def to_T(self, s, dstT, t, dkey):
        pi = self.psum()
        pk = ("ps", pi)
        pv = self.PS[pi][:].bitcast(BF16)
        for k in range(8):
            self.TR(pv[:, k * 128:(k + 1) * 128], self.xb[s][:, k * 128:(k + 1) * 128], self.ident[:],
                    r=[("xb", s), "ident"], w=[pk])
        self.CP("dve", dstT[:, :, t * 128:(t + 1) * 128], pv.rearrange("p (k t) -> p k t", k=8), r=[pk], w=[dkey])

    def proj_fm(self, wbfv, wkey, ct, tb, evac):
        pi = self.psum()
        pk = ("ps", pi)
        for k in range(8):
            self.MM(self.PS[pi][:, :], wbfv[:, k, ct * 128:(ct + 1) * 128], self.xT[:, k, tb * 512:(tb + 1) * 512],
                    k == 0, k == 7, r=[wkey, ("xT", tb)], w=[pk])
        evac(pi, pk)

    def win_pieces(self, w2d, cols):
        pieces = []
        off = 0
        for (c0, n) in cols:
            src = w2d[:, c0:c0 + n].rearrange("(k p) c -> p k c", p=128)
            pieces.append(((lambda v, off=off, n=n: v[:, :, off:off + n]), src))
            off += n
        return pieces, off

    def mem_kv(self, wmkv):
        for g in range(2):
            pieces, n = self.win_pieces(wmkv, [(g * 256, 256)])
            wv, wk = self.wload(pieces, [128, 8, 256])
            for ct in range(2):
                h = g * 2 + ct
                pi = self.psum(); pk = ("ps", pi)
                for k in range(8):
                    self.MM(self.PS[pi][:, 0:N_MEM], wv[:, k, ct * 128:(ct + 1) * 128], self.memT[:, k, :], k == 0, k == 7,
                            r=[wk, ("memT", 0), ("memT", 1)], w=[pk])
                self.CP("act", self.kTm[:, h, :], self.PS[pi][:, 0:N_MEM], r=[pk], w=[("kTm", h)])
        for g in range(2):
            pieces, n = self.win_pieces(wmkv, [(512 + g * 256, 256)])
            wv, wk = self.wload(pieces, [128, 8, 256])
            for c in range(2):
                pi = self.psum(); pk = ("ps", pi)
                for k in range(8):
                    self.MM(self.PS[pi][:, 0:256], self.memT[:, k, c * 128:(c + 1) * 128], wv[:, k, :], k == 0, k == 7,
                            r=[wk, ("memT", c)], w=[pk])
                self.CP("act", self.vm[:, c, g * 256:(g + 1) * 256], self.PS[pi][:, 0:256], r=[pk], w=[("vm", c, g)])

    def _ak(self, slot, scr):
        sm = scr["sm"]
        sid = (scr["id"], slot)
        c0s = 16 + 8 * slot
        cols = [sm[:, c0s + i:c0s + i + 1] for i in range(4)]
        keys = [("smc", sid, i) for i in range(4)]
        return sid, cols, keys

    def at_qk(self, job, slot, scr):
        qT, qkeys, kT, kkeys, nk, bias = job["qT"], job["qkeys"], job["kT"], job["kkeys"], job["nk"], job["bias"]
        sid, cols, keys = self._ak(slot, scr)
        nch = (nk + 511) // 512
        if bias is not None:
            Dsl, ch, M, mkeys = bias
            z = scr["z"][slot]
            zk = ("z", sid)
            for c in range(nch):
                c0 = c * 512
                n = min(512, nk - c0)
                pi = self.psum(); pk = ("ps", pi)
                self.MM(self.PS[pi][:, 0:n], self.ident[:, :], M[:, c0:c0 + n], True, False, r=["ident"] + list(mkeys), w=[pk])
                self.MM(self.PS[pi][:, 0:n], qT, kT[:, c0:c0 + n], False, True, r=list(qkeys) + list(kkeys), w=[pk])
                self.STT("dve", z[:, c0:c0 + n], Dsl[:, c0:c0 + n], ch, self.PS[pi][:, 0:n], ALU.mult, ALU.add,
                         r=[pk, "dtab"], w=[zk])
        else:
            pi = self.psum(); pk = ("ps", pi)
            self.MM(self.PS[pi][:, 0:nk], qT, kT[:, 0:nk], True, True, r=list(qkeys) + list(kkeys), w=[pk])
            job["_ps"] = pi

    def at_max(self, job, slot, scr):
        nk = job["nk"]
        sid, (cmx, cnm, crs, cri), (kmx, knm, krs, kri) = self._ak(slot, scr)
        p = scr["p"][slot]
        pkey = ("p", sid)
        if job["bias"] is not None:
            src, sk = scr["z"][slot][:, 0:nk], ("z", sid)
        else:
            src, sk = self.PS[job["_ps"]][:, 0:nk], ("ps", job["_ps"])
        self.TS("dve", p[:, 0:nk], src, 0.0, None, ALU.add, ALU.max, r=[sk], w=[pkey, kmx], accum=cmx)
        self.TS("dve", cnm, cmx, -SCALE, None, ALU.mult, None, r=[kmx], w=[knm])

    def at_exp(self, job, slot, scr):
        nk = job["nk"]
        sid, (cmx, cnm, crs, cri), (kmx, knm, krs, kri) = self._ak(slot, scr)
        p = scr["p"][slot]
        pkey = ("p", sid)
        if job["bias"] is not None:
            src, sk = scr["z"][slot][:, 0:nk], ("z", sid)
        else:
            src, sk = self.PS[job["_ps"]][:, 0:nk], ("ps", job["_ps"])
        self.ACT(p[:, 0:nk], src, AF.Exp, r=[sk, knm], w=[pkey, krs], bias=cnm, scale=SCALE, accum=crs)

    def at_tr(self, job, slot, scr):
        nk = job["nk"]
        sid, cols, keys = self._ak(slot, scr)
        p = scr["p"][slot]
        pkey = ("p", sid)
        nc128 = nk // 128
        banks = []
        for b0 in range(0, nc128, 8):
            nb = min(8, nc128 - b0)
            pi = self.psum(); pk = ("ps", pi)
            pv = self.PS[pi][:].bitcast(BF16)
            for c in range(nb):
                self.TR(pv[:, c * 128:(c + 1) * 128], p[:, (b0 + c) * 128:(b0 + c + 1) * 128], self.ident[:],
                        r=[pkey, "ident"], w=[pk])
            banks.append((b0, nb, pi))
        job["_tb"] = banks

    def at_evac(self, job, slot, scr):
        pT = scr["pT"]
        ptk = ("pT", scr["id"])
        for (b0, nb, pi) in job["_tb"]:
            pv = self.PS[pi][:].bitcast(BF16)
            self.CP("act", pT[:, b0:b0 + nb, :], pv[:, 0:nb * 128].rearrange("p (c t) -> p c t", c=nb), r=[("ps", pi)], w=[(ptk, b0)])

    def at_pv(self, job, slot, scr):
        nk, vfn, vkeys = job["nk"], job["vfn"], job["vkeys"]
        pT = scr["pT"]
        ptk = ("pT", scr["id"])
        nc128 = nk // 128
        pi = self.psum(); pk = ("ps", pi)
        for c in range(nc128):
            self.MM(self.PS[pi][:, 0:128], pT[:, c, :], vfn(c), c == 0, c == nc128 - 1,
                    r=[(ptk, (c // 8) * 8)] + list(vkeys), w=[pk])
        job["_pv"] = pi

    def at_fin(self, job, slot, scr):
        sid, (cmx, cnm, crs, cri), (kmx, knm, krs, kri) = self._ak(slot, scr)
        self.S.op("dve", (lambda e, cri=cri, crs=crs: e.reciprocal(cri, crs)), r=[krs], w=[kri])
        pi = job["_pv"]
        self.TS("dve", job["ydst"], self.PS[pi][:, 0:128], cri, None, ALU.mult, None, r=[("ps", pi), kri], w=[job["ydkey"]])
        if job.get("post") is not None:
            job["post"]()

    def attn_pipeline_gen(self, jobs, scr):
        n = len(jobs)
        for i in range(n + 1):
            cur = jobs[i] if i < n else None
            prv = jobs[i - 1] if i >= 1 else None
            cs, ps_ = i % 2, (i - 1) % 2
            if cur is not None:
                self.at_qk(cur, cs, scr)
            if prv is not None:
                self.at_tr(prv, ps_, scr)
                self.at_evac(prv, ps_, scr)
            if cur is not None:
                self.at_max(cur, cs, scr)
                self.at_exp(cur, cs, scr)
            if prv is not None:
                self.at_pv(prv, ps_, scr)
                self.at_fin(prv, ps_, scr)
            yield

    def attn_pipeline(self, jobs, scr, side=None, per=1):
        for _ in self.attn_pipeline_gen(jobs, scr):
            if side is not None:
                for _k in range(per):
                    next(side, None)

    def ytok_to_QY(self, ytok, ykeys, f0, nf, qt):
        for b0 in range(0, nf, 8):
            nb = min(8, nf - b0)
            pi = self.psum(); pk = ("ps", pi)
            pv = self.PS[pi][:].bitcast(BF16)
            for c in range(nb):
                self.TR(pv[:, c * 128:(c + 1) * 128], ytok[:, (b0 + c) * 128:(b0 + c + 1) * 128], self.ident[:],
                        r=list(ykeys) + ["ident"], w=[pk])
            self.CP("act", self.QY[:, f0 + b0:f0 + b0 + nb, qt * 128:(qt + 1) * 128],
                    pv[:, 0:nb * 128].rearrange("p (c t) -> p c t", c=nb), r=[pk],
                    w=[("QY", f0 + b0 + c, qt) for c in range(nb)])

    def mem_attention(self, scr, ytok):
        jobs = []
        for qt in range(16):
            for h in range(4):
                job = dict(qT=self.QY[:, 12 + h, qt * 128:(qt + 1) * 128], qkeys=[("QY", 12 + h, qt)],
                           kT=self.kTm[:, h, :], kkeys=[("kTm", h)], nk=N_MEM,
                           vfn=(lambda c, h=h: self.vm[:, c, h * 128:(h + 1) * 128]),
                           vkeys=[("vm", 0, 0), ("vm", 0, 1), ("vm", 1, 0), ("vm", 1, 1)],
                           bias=None, ydst=ytok[:, h * 128:(h + 1) * 128], ydkey=("ytok", h), post=None)
                if h == 3:
                    job["post"] = (lambda qt=qt: self.ytok_to_QY(ytok, [("ytok", hh) for hh in range(4)], 12, 4, qt))
                jobs.append(job)
        return self.attn_pipeline_gen(jobs, scr)

    def layer_A(self, j):
        nc = self.nc
        S = self.S
        w_in = self.a_w_in[j]
        with ExitStack() as st:
            sb = lambda name, shape, dtype: st.enter_context(nc.sbuf_tensor(self.nm(name), shape, dtype))
            hbuf = [sb("h%d" % i, [128, 2080], BF16) for i in range(2)]
            dg = [sb("dg%d" % i, [128, CONV_K, 128], BF16) for i in range(2)]
            sig = [sb("sig%d" % i, [128, 512], F32) for i in range(2)]
            spA = sb("spA", [128, 36 + 12 * CONV_K], F32)
            stt = [sb("stt%d" % i, [128, 512], F32) for i in range(4)]
            sq = [sb("sq%d" % i, [128, 512], BF16) for i in range(2)]
            scr = {"id": "A", "p": [sb("pA%d" % i, [128, 256], BF16) for i in range(2)],
                   "pT": sb("pTA", [128, 2, 128], BF16), "sm": self.sm}
            ytok = sb("ytokA", [128, 512], BF16)

            self.DMA(spA[:], self.a_sp[j], "spA", r=[], w=["spA"])
            for i in range(2):
                self.MEMSET("dve", hbuf[i][:, 0:32], 0.0, w=[("h", i)])
            self.mem_kv(self.a_w_mkv[j])

            for g in range(2):
                pieces, n = self.win_pieces(w_in, [(3072 + g * 256, 256)])
                wv, wk = self.wload(pieces, [128, 8, 256])
                for ct in range(2):
                    f = 12 + g * 2 + ct
                    for tb in range(4):
                        def ev(pi, pk, f=f, tb=tb):
                            self.CP("act", self.QY[:, f, tb * 512:(tb + 1) * 512], self.PS[pi][:, :], r=[pk],
                                    w=[("QY", f, tb * 4 + q) for q in range(4)])
                        self.proj_fm(wv, wk, ct, tb, ev)

            mgen = self.mem_attention(scr, ytok)
            for c in range(12):
                hs = c % 2
                hk = ("h", hs)
                pieces, n = self.win_pieces(w_in, [(c * 128, 128), (1536 + c * 128, 128)])
                wv, wk = self.wload(pieces, [128, 8, 256])
                self.TT("dve", dg[hs][:, :, :],
                        self.identf[:, :].unsqueeze(1).to_broadcast([128, CONV_K, 128]),
                        spA[:, 36 + c * CONV_K:36 + (c + 1) * CONV_K].unsqueeze(2).to_broadcast([128, CONV_K, 128]),
                        ALU.mult, r=["identf", "spA"], w=[("dg", hs)])
                for tb in range(4):
                    ss = tb % 2
                    def ev_g(pi, pk, ss=ss):
                        self.ACT(sig[ss][:, :], self.PS[pi][:, :], AF.Sigmoid, r=[pk], w=[("sig", ss)])
                    def ev_a(pi, pk, ss=ss, tb=tb, hs=hs, hk=hk):
                        self.TT("dve", hbuf[hs][:, 30 + tb * 512:30 + (tb + 1) * 512], self.PS[pi][:, :], sig[ss][:, :],
                                ALU.mult, r=[pk, ("sig", ss)], w=[(hk, tb)])
                    self.proj_fm(wv, wk, 1, tb, ev_g)
                    self.proj_fm(wv, wk, 0, tb, ev_a)
                for tb in range(4):
                    pi = self.psum(); pk = ("ps", pi)
                    rk = [hk, ("dg", hs)] + [(hk, t2) for t2 in range(max(0, tb - 1), tb + 1)]
                    for k in range(CONV_K):
                        self.MM(self.PS[pi][:, :], dg[hs][:, k, :], hbuf[hs][:, tb * 512 + k:tb * 512 + k + 512],
                                k == 0, k == CONV_K - 1, r=rk, w=[pk])
                    self.ACT(self.QY[:, c, tb * 512:(tb + 1) * 512], self.PS[pi][:, :], AF.Identity, r=[pk, "spA"],
                             w=[("QY", c, tb * 4 + q) for q in range(4)], bias=spA[:, c:c + 1], scale=1.0)
            for _ in mgen:
                pass

            for tb in range(4):
                tsl = slice(tb * 512, (tb + 1) * 512)
                p1 = self.psum(); p2 = self.psum()
                for c in range(12):
                    qk = [("QY", c, tb * 4 + q) for q in range(4)]
                    ss = c % 2
                    self.ACT(sq[ss][:, :], self.QY[:, c, tsl], AF.Square, r=qk, w=[("sq", ss)])
                    self.MM(self.PS[p1][:, :], self.ones[:, :], self.QY[:, c, tsl], c == 0, c == 11, r=qk + ["ones"], w=[("ps", p1)])
                    self.MM(self.PS[p2][:, :], self.ones[:, :], sq[ss][:, :], c == 0, c == 11, r=[("sq", ss), "ones"], w=[("ps", p2)])
                mean, msq, var, rstd = stt
                self.ACT(mean[:, :], self.PS[p1][:, :], AF.Identity, r=[("ps", p1)], w=["stt0"], scale=1.0 / CONV_W)
                self.TT("dve", msq[:, :], mean[:, :], mean[:, :], ALU.mult, r=["stt0"], w=["stt1"])
                self.STT("dve", var[:, :], self.PS[p2][:, :], 1.0 / CONV_W, msq[:, :], ALU.mult, ALU.subtract,
                         r=[("ps", p2), "stt1"], w=["stt2"])
                self.TS("dve", var[:, :], var[:, :], LN_EPS, None, ALU.add, None, r=["stt2"], w=["stt2"])
                self.S.op("act", lambda e, var=var: e.sqrt(var[:, :], var[:, :]), r=["stt2"], w=["stt2"])
                self.S.op("dve", lambda e, var=var, rstd=rstd: e.reciprocal(rstd[:, :], var[:, :]), r=["stt2"], w=["stt3"])
                for c in range(12):
                    qk = [("QY", c, tb * 4 + q) for q in range(4)]
                    ss = c % 2
                    self.TT("dve", sig[ss][:, :], self.QY[:, c, tsl], mean[:, :], ALU.subtract, r=qk + ["stt0"], w=[("sig", ss)])
                    self.TT("dve", sig[ss][:, :], sig[ss][:, :], rstd[:, :], ALU.mult, r=[("sig", ss), "stt3"], w=[("sig", ss)])
                    self.ACT(self.QY[:, c, tsl], sig[ss][:, :], AF.Silu, r=[("sig", ss), "spA"], w=qk,
                             bias=spA[:, 24 + c:25 + c], scale=spA[:, 12 + c:13 + c])

    def layer_B(self, j):
        nc = self.nc
        S = self.S
        w_in = self.b_w_in[j]
        slopes = _alibi_slopes(ATT_H)
        with ExitStack() as st:
            sb = lambda name, shape, dtype: st.enter_context(nc.sbuf_tensor(self.nm(name), shape, dtype))
            kT = sb("kT", [128, SEQ], BF16)
            v = sb("v", [128, 16, 128], BF16)
            qiT = sb("qiT", [128, 4, SEQ], BF16)
            kiT2 = sb("kiT2", [128, SEQ], BF16)
            wi = sb("wi", [128, 16, 8], F32)
            dtab = sb("dtab", [128, SEQ], F32)
            madm = sb("madm", [128, 256], F32)
            zz = [sb("zB%d" % i, [128, SEQ], F32) for i in range(2)]
            z = zz[0]
            M = sb("MB", [128, SEQ], BF16)
            pp = [sb("pB%d" % i, [128, SEQ], BF16) for i in range(2)]
            junk = pp[0]
            pT = sb("pTB", [128, 16, 128], BF16)
            ytok = sb("ytokB", [128, CONV_W], BF16)
            rl = [sb("rl%d" % i, [128, 512], F32) for i in range(2)]
            sm = self.sm
            scr = {"id": "B", "z": zz, "p": pp, "pT": pT, "sm": sm}
            zk = ("z", ("B", 0))
            jk = ("p", ("B", 0))

            self.DMA(dtab[:], self.c_dtab[:, :], "cst", r=[], w=["dtab"])
            self.DMA(madm[:], self.c_madm[:, :], "cst", r=[], w=["madm"])

            def fm_group(cols, dests, side=None):
                pieces, n = self.win_pieces(w_in, cols)
                wv, wk = self.wload(pieces, [128, 8, n])
                for ct, (dst_fn, key_fn) in enumerate(dests):
                    for tb in range(4):
                        def ev(pi, pk, dst_fn=dst_fn, key_fn=key_fn, tb=tb):
                            self.CP("act", dst_fn(tb), self.PS[pi][:, :], r=[pk], w=key_fn(tb))
                        self.proj_fm(wv, wk, ct, tb, ev)
                        if side is not None:
                            next(side, None)
                            next(side, None)

            def qy_dst(f):
                return ((lambda tb: self.QY[:, f, tb * 512:(tb + 1) * 512]),
                        (lambda tb: [("QY", f, tb * 4 + q) for q in range(4)]))

            fm_group([(1536, 128), (2304, 64), (2304, 64)],
                     [((lambda tb: kT[:, tb * 512:(tb + 1) * 512]), (lambda tb: [("kT", tb)])),
                      ((lambda tb: kiT2[:, tb * 512:(tb + 1) * 512]), (lambda tb: [("kiT2", tb)]))])
            pieces, n = self.win_pieces(w_in, [(1664, 128), (2368, 8)])
            wv, wk = self.wload(pieces, [128, 8, 136])
            for t in range(16):
                pi = self.psum(); pk = ("ps", pi)
                for k in range(8):
                    self.MM(self.PS[pi][:, 0:136], self.xT[:, k, t * 128:(t + 1) * 128], wv[:, k, :], k == 0, k == 7,
                            r=[wk, ("xT", t // 4)], w=[pk])
                self.CP("act", v[:, t, :], self.PS[pi][:, 0:128], r=[pk], w=[("v", t)])
                self.CP("act", wi[:, t, :], self.PS[pi][:, 128:136], r=[pk], w=[("wi", t)])
            for g in range(2):
                fm_group([(1792 + g * 256, 256)],
                         [((lambda tb, f=g * 2 + c: qiT[:, f, tb * 512:(tb + 1) * 512]),
                           (lambda tb, f=g * 2 + c: [("qiT", f, tb)])) for c in range(2)])
            for g in range(2):
                fm_group([(2376 + g * 256, 256)], [qy_dst(12 + g * 2), qy_dst(12 + g * 2 + 1)])
            self.mem_kv(self.b_w_mkv[j])
            mgen = self.mem_attention(scr, ytok)
            for g in range(6):
                fm_group([(g * 256, 256)], [qy_dst(g * 2), qy_dst(g * 2 + 1)])
            for _ in mgen:
                pass

            cA, cLO, cW, cMID, cCNT, cG = 8, 9, 10, 11, 12, 13
            col = lambda c: sm[:, c:c + 1]
            ck = lambda c: ("smb", c)
            isc = self.wst[1]
            isck = ("wst", 1)
            junk = self.wbf[0]
            jk = ("wbf", 0)
            Mb = [M, self.wbf[1]]
            Mk = [[("M", 0)], [("M", 1), ("wbf", 1)]]

            def mask_gen(qt):
                nk = 128 * (qt + 1)
                nch = (nk + 511) // 512
                Mq = Mb[qt % 2]
                mk = Mk[qt % 2]
                if qt < 2:
                    if nk > 128:
                        self.MEMSET("dve", Mq[:, 0:nk - 128], 0.0, w=mk)
                    self.CP("dve", Mq[:, nk - 128:nk], madm[:, 0:128], r=["madm"], w=mk)
                    yield
                    return
                for h in range(IDX_H):
                    hb = (h % 2) * 64
                    for c in range(nch):
                        c0 = c * 512
                        n = min(512, nk - c0)
                        pi = self.psum(); pk = ("ps", pi)
                        ss = (h * nch + c) % 2
                        self.MM(self.PS[pi][:, 0:n], qiT[hb:hb + 64, h // 2, qt * 128:(qt + 1) * 128], kiT2[hb:hb + 64, c0:c0 + n],
                                True, True, r=[("qiT", h // 2, qt // 4), ("kiT2", c)], w=[pk])
                        self.ACT(rl[ss][:, 0:n], self.PS[pi][:, 0:n], AF.Relu, r=[pk], w=[("rl", ss)])
                        if h == 0:
                            self.TS("dve", isc[:, c0:c0 + n], rl[ss][:, 0:n], wi[:, qt, 0:1], None, ALU.mult, None,
                                    r=[("rl", ss), ("wi", qt)], w=[isck])
                        else:
                            self.STT("dve", isc[:, c0:c0 + n], rl[ss][:, 0:n], wi[:, qt, h:h + 1], isc[:, c0:c0 + n], ALU.mult, ALU.add,
                                     r=[("rl", ss), ("wi", qt), isck], w=[isck])
                        yield
                self.TS("dve", junk[:, 0:nk], isc[:, 0:nk], 0.0, None, ALU.add, ALU.max, r=[isck], w=[jk, ck(cA)], accum=col(cA))
                self.TS("dve", junk[:, 0:nk], isc[:, 0:nk], 0.0, None, ALU.add, ALU.min, r=[isck], w=[jk, ck(cLO)], accum=col(cLO))
                yield
                self.TT("dve", col(cW), col(cA), col(cLO), ALU.subtract, r=[ck(cA), ck(cLO)], w=[ck(cW)])
                self.TS("dve", col(cW), col(cW), 1.000002, 1e-20, ALU.mult, ALU.add, r=[ck(cW)], w=[ck(cW)])
                self.TT("dve", isc[:, nk - 128:nk], isc[:, nk - 128:nk], madm[:, 128:256], ALU.add, r=[isck, "madm"], w=[isck])
                yield
                for it in range(NBISECT):
                    cn = 2.0 ** (-(it + 1))
                    self.STT("dve", col(cMID), col(cW), cn, col(cLO), ALU.mult, ALU.add, r=[ck(cW), ck(cLO)], w=[ck(cMID)])
                    self.TS("dve", junk[:, 0:nk], isc[:, 0:nk], col(cMID), None, ALU.is_ge, ALU.add, r=[isck, ck(cMID)],
                            w=[jk, ck(cCNT)], accum=col(cCNT))
                    self.TS("dve", col(cG), col(cCNT), float(TOPK) - 0.5, cn, ALU.is_ge, ALU.mult, r=[ck(cCNT)], w=[ck(cG)])
                    self.STT("dve", col(cLO), col(cG), col(cW), col(cLO), ALU.mult, ALU.add, r=[ck(cG), ck(cW), ck(cLO)], w=[ck(cLO)])
                    yield
                self.TS("dve", Mq[:, 0:nk], isc[:, 0:nk], col(cLO), MASKNEG, ALU.is_lt, ALU.mult, r=[isck, ck(cLO)], w=mk)
                yield

            for _ in mask_gen(0):
                pass
            for qt in range(16):
                nk = 128 * (qt + 1)
                nch = (nk + 511) // 512
                d0 = 1920 - 128 * qt
                jobs = []
                for h in range(ATT_H):
                    jobs.append(dict(qT=self.QY[:, h, qt * 128:(qt + 1) * 128], qkeys=[("QY", h, qt)],
                                     kT=kT[:, 0:nk], kkeys=[("kT", c) for c in range(nch)], nk=nk,
                                     vfn=(lambda c: v[:, c, :]), vkeys=[("v", c) for c in range(nk // 128)],
                                     bias=(dtab[:, d0:d0 + nk], -slopes[h] / SCALE, Mb[qt % 2], [("M", qt % 2)]),
                                     ydst=ytok[:, h * 128:(h + 1) * 128], ydkey=("ytok", h), post=None))
                side = mask_gen(qt + 1) if qt < 15 else None
                self.attn_pipeline(jobs, scr, side=side, per=5)
                if side is not None:
                    for _ in side:
                        pass
                self.ytok_to_QY(ytok, [("ytok", h) for h in range(ATT_H)], 0, ATT_H, qt)

    def gate_out(self, wgate, goff, wout, pgbsrc, xsrc, xdst, last, li):
        nc = self.nc
        S = self.S
        with ExitStack() as st:
            sb = lambda name, shape, dtype: st.enter_context(nc.sbuf_tensor(self.nm(name), shape, dtype))
            woutb = sb("woutb", [128, 16, D_MODEL], BF16)
            self.xio = [sb("xio%d" % i, [128, D_MODEL], F32) for i in range(2)]
            self.xb = [sb("xb%d" % i, [128, D_MODEL], BF16) for i in range(2)]
            self.pgb = sb("pgb", [128, 2 * D_MODEL], F32)
            self.lnst = sb("lnst", [128, 12], F32)
            self.lnag = sb("lnag", [128, 4], F32)
            gt = [sb("gt%d" % i, [128, 512], BF16) for i in range(2)]
            self.DMA(self.pgb[:], pgbsrc, "pgb", r=[], w=["pgb"])
            for g in range(8):
                pieces, n = self.win_pieces(wgate, [(goff + g * 256, 256)])
                wv, wk = self.wload(pieces, [128, 8, 256])
                for ct in range(2):
                    f = g * 2 + ct
                    for tb in range(4):
                        def ev(pi, pk, f=f, tb=tb):
                            ss = tb % 2
                            qk = [("QY", f, tb * 4 + q) for q in range(4)]
                            self.ACT(gt[ss][:, :], self.PS[pi][:, :], AF.Silu, r=[pk], w=[("gt", ss)])
                            self.TT("dve", self.QY[:, f, tb * 512:(tb + 1) * 512], self.QY[:, f, tb * 512:(tb + 1) * 512],
                                    gt[ss][:, :], ALU.mult, r=qk + [("gt", ss)], w=qk)
                        self.proj_fm(wv, wk, ct, tb, ev)
                s = self.w_rr % 2
                self.w_rr += 1
                stv = self.wst[s][:, :].rearrange("p (a b) -> p a b", a=2)
                self.DMA(stv, wout[g * 256:(g + 1) * 256, :].rearrange("(k p) c -> p k c", p=128), "wst%d" % s, r=[], w=[("wst", s)])
                self.CP("pool", woutb[:, 2 * g:2 * g + 2, :], stv, r=[("wst", s)], w=[("wout", g)])
            for t in range(16):
                s = t % 2
                xk = ("xio", s)
                self.DMA(self.xio[s][:], xsrc[t * 128:(t + 1) * 128, :], "xio%d" % s, r=[("xdram", li - 1, t)], w=[xk])
                for half in range(2):
                    pi = self.psum(); pk = ("ps", pi)
                    for f in range(16):
                        self.MM(self.PS[pi][:, :], self.QY[:, f, t * 128:(t + 1) * 128], woutb[:, f, half * 512:(half + 1) * 512],
                                f == 0, f == 15, r=[("QY", f, t), ("wout", f // 2)], w=[pk])
                    self.STT("dve", self.xio[s][:, half * 512:(half + 1) * 512], self.xio[s][:, half * 512:(half + 1) * 512], ALPHA,
                             self.PS[pi][:, :], ALU.mult, ALU.add, r=[pk, xk], w=[xk])
                self.layernorm_tile(s)
                self.DMA(xdst[t * 128:(t + 1) * 128, :], self.xio[s][:], "xout%d" % s, r=[xk], w=[("xdram", li, t)])
                if not last:
                    self.to_T(s, self.xT, t, ("xT", t // 4))


_PROG_CACHE = {}


def _get_prog(layers):
    key = tuple(layers)
    if key not in _PROG_CACHE:
        p = Prog(layers)
        p.build()
        _PROG_CACHE[key] = p
    return _PROG_CACHE[key]


def _consts():
    ident = np.eye(128, dtype=np.float32)
    p = np.arange(128, dtype=np.float32)[:, None]
    jj = np.arange(SEQ, dtype=np.float32)[None, :]
    dtab = np.abs(p - (jj - 1920.0)).astype(np.float32)
    madm = np.zeros((128, 256), dtype=np.float32)
    madm[0:64, 64:128] = MASKNEG
    madm[0:64, 128 + 64:256] = -1e30
    return ident, dtab, madm


def _host_inputs(inp, b):
    f = lambda a: np.ascontiguousarray(np.asarray(a, dtype=np.float32))
    ident, dtab, madm = _consts()
    rep = lambda v: np.ascontiguousarray(np.broadcast_to(np.asarray(v, np.float32)[None, :], (128, v.shape[-1])))
    a_sp = np.zeros((2, 128, 36 + 12 * CONV_K), np.float32)
    for j in range(2):
        a_sp[j, :, 0:12] = np.asarray(inp["a_conv_b"][j]).reshape(12, 128).T
        a_sp[j, :, 12:24] = np.asarray(inp["a_ln_g"][j]).reshape(12, 128).T
        a_sp[j, :, 24:36] = np.asarray(inp["a_ln_b"][j]).reshape(12, 128).T
        cw = np.asarray(inp["a_conv_w"][j]).reshape(CONV_K, 12, 128)
        a_sp[j, :, 36:] = np.transpose(cw, (2, 1, 0)).reshape(128, 12 * CONV_K)
    a_pgb = np.stack([np.concatenate([rep(inp["a_post_g"][j]), rep(inp["a_post_b"][j])], axis=1) for j in range(2)])
    b_pgb = np.stack([np.concatenate([rep(inp["b_post_g"][j]), rep(inp["b_post_b"][j])], axis=1) for j in range(2)])
    memgb = np.concatenate([rep(np.asarray(inp["mem_ln_g"])), rep(np.asarray(inp["mem_ln_b"]))], axis=1)
    return {
        "x": f(inp["x"][b]), "mem": f(inp["mem"][b]), "memgb": f(memgb),
        "c_ident": ident, "c_dtab": dtab, "c_madm": madm,
        "a_w_in": f(inp["a_w_in"]), "a_sp": f(a_sp), "a_w_mkv": f(inp["a_w_mkv"]), "a_w_out": f(inp["a_w_out"]),
        "a_pgb": f(a_pgb),
        "b_w_in": f(inp["b_w_in"]), "b_w_mkv": f(inp["b_w_mkv"]), "b_w_out": f(inp["b_w_out"]), "b_pgb": f(b_pgb),
    }


def run_layers(inp, layers, cores):
    prog = _get_prog(layers)
    maps = [_host_inputs(inp, b) for b in cores]
    shared = {}
    for m in maps[1:]:
        for k in m:
            if k not in ("x", "mem"):
                m[k] = maps[0][k]
    res = run_bass_kernel_spmd(prog.nc, maps, core_ids=list(range(len(cores))))
    if DEBUG:
        global _DBG
        _DBG = [{k: np.asarray(r[k]) for k in ("dbg", "dbg_k", "dbg_v", "dbg_m")} for r in res.results]
    return np.stack([r["out"] for r in res.results], axis=0)


def kernel(**inputs):
    inp = {k: np.asarray(v) for k, v in inputs.items()}
    out = run_layers(inp, [0, 1, 2, 3], list(range(8)))
    return out.astype(np.float32)
```

```python
import numpy as np
from contextlib import ExitStack
import concourse.bass as bass
import concourse.mybir as mybir
from concourse.bass_utils import run_bass_kernel_spmd

F32 = mybir.dt.float32
BF16 = mybir.dt.bfloat16
AF = mybir.ActivationFunctionType
ALU = mybir.AluOpType
AX = mybir.AxisListType

D_MODEL = 1024
SEQ = 2048
DEPTH = 4
N_MEM = 256
HD = 128
CONV_W = 1536
CONV_K = 31
ATT_H = 12
IDX_H = 8
IDX_D = 64
TOPK = 256
ALPHA = (2 * DEPTH) ** 0.25
LN_EPS = 1e-5
SCALE = HD ** -0.5
A_IN = 5632
B_IN = 4936
NBISECT = 20
DEBUG = False
MASKNEG = -30000.0


def _alibi_slopes(n):
    import math
    p = 2 ** int(math.floor(math.log2(n)))
    base = [2.0 ** (-8.0 * (i + 1) / p) for i in range(p)]
    extra = [2.0 ** (-4.0 * (2 * i + 1) / p) for i in range(n - p)]
    return base + extra


class _Op:
    __slots__ = ("eng", "fn", "deps", "dma", "sig", "cnt", "waits", "clock", "gid")


class Sched:
    ENG = ("pe", "act", "dve", "pool", "sp")

    def __init__(self):
        self.ops = []
        self.lastw = {}
        self.readers = {}
        self.streams = {}
        self.last_on = {}
        self.pending_dma = []

    def op(self, eng, fn, r=(), w=(), dma=None, extra=()):
        o = _Op()
        o.eng = eng; o.fn = fn; o.dma = dma; o.sig = False; o.gid = len(self.ops)
        o.cnt = 0; o.clock = None; o.waits = ()
        deps = {}

        def add(d, kind):
            if d is None or d is o:
                return
            if d.dma is None and dma is None and d.eng == eng:
                if eng == "pe":
                    return
                if kind == "war":
                    return
            deps[d.gid] = d

        for k in r:
            add(self.lastw.get(k), "raw")
            if isinstance(k, tuple) and k[0] == "ps":
                rd = self.readers.get(k)
                if rd:
                    for x in rd.values():
                        if x.eng != eng:
                            add(x, "raw")
        for k in w:
            add(self.lastw.get(k), "waw")
            rd = self.readers.get(k)
            if rd:
                for x in rd.values():
                    add(x, "war")
        for d in extra:
            add(d, "raw")
        for k in w:
            self.lastw[k] = o
            self.readers[k] = {}
        for k in r:
            rd = self.readers.setdefault(k, {})
            rd[("dma", o.gid) if dma is not None else eng] = o
        o.deps = list(deps.values())
        self.ops.append(o)
        self.last_on[eng] = o
        if dma is not None:
            self.streams[dma] = self.streams.get(dma, 0) + 1
            o.cnt = self.streams[dma] * 16
            self.pending_dma.append(o)
        return o

    def barrier(self):
        last = [self.last_on[e] for e in self.ENG if e in self.last_on]
        dmas = list(self.pending_dma)
        self.pending_dma = []
        for e in self.ENG:
            ex = [d for d in last if d.eng != e or d.dma is not None] + dmas
            self.op(e, (lambda en: en.nop()), extra=ex)

    def finalize(self):
        for o in self.ops:
            for d in o.deps:
                d.sig = True
        cnt = {e: 0 for e in self.ENG}
        for o in self.ops:
            if o.dma is None and o.sig:
                cnt[o.eng] += 1
                o.cnt = cnt[o.eng]
        eclock = {e: {} for e in self.ENG}
        for o in self.ops:
            ck = eclock[o.eng]
            waits = {}
            for d in sorted(o.deps, key=lambda t: t.gid):
                key = d.dma if d.dma is not None else d.eng
                if ck.get(key, 0) >= d.cnt:
                    continue
                waits[key] = max(waits.get(key, 0), d.cnt)
                for k2, v2 in d.clock.items():
                    if ck.get(k2, 0) < v2:
                        ck[k2] = v2
            o.waits = tuple(waits.items())
            if o.dma is not None:
                c2 = dict(ck); c2[o.dma] = max(c2.get(o.dma, 0), o.cnt); o.clock = c2
            elif o.sig:
                c2 = dict(ck); c2[o.eng] = o.cnt; o.clock = c2
        self.byeng = {e: [o for o in self.ops if o.eng == e] for e in self.ENG}

    def emit(self, nc, stack):
        self.finalize()
        sems = {}
        for key in list(self.ENG) + list(self.streams.keys()):
            sems[key] = stack.enter_context(nc.semaphore("s_" + str(key)))
        streams = self.streams
        byeng = self.byeng

        def runner(engname):
            def f(e):
                for o in byeng[engname]:
                    for key, val in o.waits:
                        e.wait_ge(sems[key], val)
                    ins = o.fn(e)
                    if o.dma is not None:
                        ins.then_inc(sems[o.dma], 16)
                    elif o.sig:
                        ins.then_inc(sems[o.eng], 1)
                if engname == "sp":
                    for key, n in streams.items():
                        e.wait_ge(sems[key], 16 * n)
            return f

        with nc.Block() as block:
            block.tensor(runner("pe"))
            block.scalar(runner("act"))
            block.vector(runner("dve"))
            block.gpsimd(runner("pool"))
            block.sync(runner("sp"))


class Prog:
    def __init__(self, layers, final_to_out=True):
        self.layers = list(layers)
        self.S = Sched()
        self.nc = bass.Bass("TRN2", target_bir_lowering=False)
        self.ps_rr = 0
        self.uid = 0

    def nm(self, name):
        self.ncnt = getattr(self, "ncnt", 0) + 1
        return "%s_%d" % (name, self.ncnt)

    def MM(self, out, lhsT, rhs, start, stop, r, w):
        return self.S.op("pe", lambda e: e.matmul(out, lhsT, rhs, start=start, stop=stop), r=r, w=w)

    def TR(self, out, in_, ident, r, w):
        return self.S.op("pe", lambda e: e.transpose(out, in_, ident), r=r, w=w)

    def ACT(self, out, in_, func, r, w, bias=None, scale=None, accum=None):
        kw = {}
        if bias is not None:
            kw["bias"] = bias
        if scale is not None:
            kw["scale"] = scale
        if accum is not None:
            kw["accum_out"] = accum
        return self.S.op("act", lambda e: e.activation(out=out, in_=in_, func=func, **kw), r=r, w=w)

    def TS(self, eng, out, in0, s1, s2, op0, op1, r, w, accum=None):
        kw = {}
        if op1 is not None:
            kw["op1"] = op1
        if accum is not None:
            kw["accum_out"] = accum
        return self.S.op(eng, lambda e: e.tensor_scalar(out=out, in0=in0, scalar1=s1, scalar2=s2, op0=op0, **kw), r=r, w=w)

    def TT(self, eng, out, in0, in1, op, r, w):
        return self.S.op(eng, lambda e: e.tensor_tensor(out=out, in0=in0, in1=in1, op=op), r=r, w=w)

    def STT(self, eng, out, in0, scalar, in1, op0, op1, r, w):
        return self.S.op(eng, lambda e: e.scalar_tensor_tensor(out=out, in0=in0, scalar=scalar, in1=in1, op0=op0, op1=op1), r=r, w=w)

    def CP(self, eng, out, in_, r, w):
        if eng == "act":
            return self.S.op("act", lambda e: e.copy(out=out, in_=in_), r=r, w=w)
        return self.S.op(eng, lambda e: e.tensor_copy(out=out, in_=in_), r=r, w=w)

    def MEMSET(self, eng, ap, val, w):
        return self.S.op(eng, lambda e: e.memset(ap, val), w=w)

    def DMA(self, out, in_, stream, r, w):
        return self.S.op("sp", lambda e: e.dma_start(out=out, in_=in_), r=r, w=w, dma=stream)

    def psum(self):
        i = self.ps_rr
        self.ps_rr = (self.ps_rr + 1) % 8
        return i

    def wload(self, pieces, shape):
        s = self.w_rr % 2
        b = self.wb_rr % 2
        self.w_rr += 1
        self.wb_rr += 1
        a, bb = shape[1], shape[2]
        n = a * bb
        assert n <= 2048
        stv = self.wst[s][:, 0:n].rearrange("p (a b) -> p a b", a=a)
        bfv = self.wbf[b][:, 0:n].rearrange("p (a b) -> p a b", a=a)
        for dst_fn, src in pieces:
            self.DMA(dst_fn(stv), src, "wst%d" % s, r=[], w=[("wst", s)])
        self.CP("pool", bfv, stv, r=[("wst", s)], w=[("wbf", b)])
        return bfv, ("wbf", b)

    def build(self):
        nc = self.nc
        S = self.S
        L = self.layers
        dt = nc.dram_tensor
        self.x_in = dt("x", [SEQ, D_MODEL], F32, kind="ExternalInput").ap()
        self.mem_in = dt("mem", [N_MEM, D_MODEL], F32, kind="ExternalInput").ap()
        self.memgb = dt("memgb", [128, 2 * D_MODEL], F32, kind="ExternalInput").ap()
        self.c_ident = dt("c_ident", [128, 128], F32, kind="ExternalInput").ap()
        self.c_dtab = dt("c_dtab", [128, SEQ], F32, kind="ExternalInput").ap()
        self.c_madm = dt("c_madm", [128, 256], F32, kind="ExternalInput").ap()
        self.a_w_in = dt("a_w_in", [2, D_MODEL, A_IN], F32, kind="ExternalInput").ap()
        self.a_sp = dt("a_sp", [2, 128, 36 + 12 * CONV_K], F32, kind="ExternalInput").ap()
        self.a_w_mkv = dt("a_w_mkv", [2, D_MODEL, 1024], F32, kind="ExternalInput").ap()
        self.a_w_out = dt("a_w_out", [2, 2048, D_MODEL], F32, kind="ExternalInput").ap()
        self.a_pgb = dt("a_pgb", [2, 128, 2 * D_MODEL], F32, kind="ExternalInput").ap()
        self.b_w_in = dt("b_w_in", [2, D_MODEL, B_IN], F32, kind="ExternalInput").ap()
        self.b_w_mkv = dt("b_w_mkv", [2, D_MODEL, 1024], F32, kind="ExternalInput").ap()
        self.b_w_out = dt("b_w_out", [2, 2048, D_MODEL], F32, kind="ExternalInput").ap()
        self.b_pgb = dt("b_pgb", [2, 128, 2 * D_MODEL], F32, kind="ExternalInput").ap()
        self.out = dt("out", [SEQ, D_MODEL], F32, kind="ExternalOutput").ap()
        self.dbg = dt("dbg", [128, 16 * SEQ], BF16, kind="ExternalOutput").ap() if DEBUG else None
        if DEBUG:
            self.dbg_k = dt("dbg_k", [128, 4 * 256], BF16, kind="ExternalOutput").ap()
            self.dbg_v = dt("dbg_v", [128, 2 * 512], BF16, kind="ExternalOutput").ap()
            self.dbg_m = dt("dbg_m", [128, 8 * 256], BF16, kind="ExternalOutput").ap()
        self.xs = [dt("xs0", [SEQ, D_MODEL], F32).ap(), dt("xs1", [SEQ, D_MODEL], F32).ap()]

        with ExitStack() as st:
            sb = lambda name, shape, dtype: st.enter_context(nc.sbuf_tensor(self.nm(name), shape, dtype))
            self.PS = [st.enter_context(nc.psum_tensor("ps%d" % i, [128, 512], F32)) for i in range(8)]
            self.identf = sb("identf", [128, 128], F32)
            self.ident = sb("ident", [128, 128], BF16)
            self.ones = sb("ones", [128, 128], BF16)
            self.memT = sb("memT", [128, 8, N_MEM], BF16)
            self.xT = sb("xT", [128, 8, SEQ], BF16)
            self.QY = sb("QY", [128, 16, SEQ], BF16)
            self.wst = [sb("wst%d" % i, [128, 2048], F32) for i in range(2)]
            self.wbf = [sb("wbf%d" % i, [128, 2048], BF16) for i in range(2)]
            self.kTm = sb("kTm", [128, 4, N_MEM], BF16)
            self.vm = sb("vm", [128, 2, 512], BF16)
            self.sm = sb("sm", [128, 64], F32)
            self.w_rr = 0
            self.wb_rr = 0

            self.DMA(self.identf[:], self.c_ident[:, :], "cst", r=[], w=["identf"])
            self.CP("dve", self.ident[:], self.identf[:], r=["identf"], w=["ident"])
            self.MEMSET("dve", self.ones[:], 1.0, w=["ones"])

            with ExitStack() as st0:
                sb0 = lambda name, shape, dtype: st0.enter_context(nc.sbuf_tensor(self.nm(name), shape, dtype))
                self.xio = [sb0("xio%d" % i, [128, D_MODEL], F32) for i in range(2)]
                self.xb = [sb0("xb%d" % i, [128, D_MODEL], BF16) for i in range(2)]
                self.pgb = sb0("pgb", [128, 2 * D_MODEL], F32)
                self.lnst = sb0("lnst", [128, 12], F32)
                self.lnag = sb0("lnag", [128, 4], F32)
                self.DMA(self.pgb[:], self.memgb[:, :], "pgb", r=[], w=["pgb"])
                for t in range(2):
                    s = t % 2
                    self.DMA(self.xio[s][:], self.mem_in[t * 128:(t + 1) * 128, :], "xio%d" % s, r=[], w=[("xio", s)])
                    self.layernorm_tile(s)
                    self.to_T(s, self.memT, t, ("memT", t))
                for t in range(16):
                    s = t % 2
                    self.DMA(self.xio[s][:], self.x_in[t * 128:(t + 1) * 128, :], "xio%d" % s, r=[], w=[("xio", s)])
                    self.CP("act", self.xb[s][:], self.xio[s][:], r=[("xio", s)], w=[("xb", s)])
                    self.to_T(s, self.xT, t, ("xT", t // 4))
            S.barrier()

            xsrc = self.x_in
            for li, layer in enumerate(L):
                j = layer // 2
                last = (li == len(L) - 1)
                xdst = self.out if last else self.xs[li % 2]
                if layer % 2 == 0:
                    self.layer_A(j)
                    wout, pgbsrc, wgate, goff = self.a_w_out[j], self.a_pgb[j], self.a_w_in[j], 3584
                else:
                    self.layer_B(j)
                    wout, pgbsrc, wgate, goff = self.b_w_out[j], self.b_pgb[j], self.b_w_in[j], 2888
                S.barrier()
                if DEBUG and li == 0:
                    self.DMA(self.dbg[:, :], self.QY[:, :, :].rearrange("p f t -> p (f t)"), "dbg", r=[("QY", f, t) for f in range(16) for t in range(16)], w=[])
                    self.DMA(self.dbg_k[:, :], self.kTm[:, :, :].rearrange("p f t -> p (f t)"), "dbg", r=[("kTm", h) for h in range(4)], w=[])
                    self.DMA(self.dbg_v[:, :], self.vm[:, :, :].rearrange("p f t -> p (f t)"), "dbg", r=[("vm", 0, 0)], w=[])
                    self.DMA(self.dbg_m[:, :], self.memT[:, :, :].rearrange("p f t -> p (f t)"), "dbg", r=[("memT", 0)], w=[])
                    S.barrier()
                self.gate_out(wgate, goff, wout, pgbsrc, xsrc, xdst, last, li)
                S.barrier()
                xsrc = xdst
            S.emit(nc, st)
        return nc

    def layernorm_tile(self, s):
        xk = ("xio", s)
        x = self.xio[s]
        lnst, lnag, pgb, xb = self.lnst, self.lnag, self.pgb, self.xb[s]
        S = self.S
        for c in range(2):
            S.op("dve", (lambda e, c=c, lnst=lnst, x=x: e.bn_stats(lnst[:, c * 6:(c + 1) * 6], x[:, c * 512:(c + 1) * 512])),
                 r=[xk], w=[("lnst", c)])
        S.op("dve", (lambda e, lnst=lnst, lnag=lnag: e.bn_aggr(lnag[:, 0:2], lnst[:, :])), r=[("lnst", 0), ("lnst", 1)], w=["lnag"])
        self.TS("dve", lnag[:, 2:3], lnag[:, 1:2], LN_EPS, None, ALU.add, None, r=["lnag"], w=["lnrs0"])
        S.op("act", (lambda e, lnag=lnag: e.sqrt(lnag[:, 2:3], lnag[:, 2:3])), r=["lnrs0"], w=["lnrs0"])
        S.op("dve", (lambda e, lnag=lnag: e.reciprocal(lnag[:, 3:4], lnag[:, 2:3])), r=["lnrs0"], w=["lnrs"])
        self.TS("dve", x[:], x[:], lnag[:, 0:1], lnag[:, 3:4], ALU.subtract, ALU.mult, r=[xk, "lnag", "lnrs"], w=[xk])
        self.TT("dve", x[:], x[:], pgb[:, 0:D_MODEL], ALU.mult, r=[xk, "pgb"], w=[xk])
        self.TT("dve", x[:], x[:], pgb[:, D_MODEL:2 * D_MODEL], ALU.add, r=[xk, "pgb"], w=[xk])
        self.CP("act", xb[:], x[:], r=[xk], w=[("xb", s)])

    def to_T(self, s, dstT, t, dkey):
        pi = self.psum()
        pk = ("ps", pi)
        pv = self.PS[pi][:].bitcast(BF16)
        for k in range(8):
            self.TR(pv[:, k * 128:(k + 1) * 128], self.xb[s][:, k * 128:(k + 1) * 128], self.ident[:],
                    r=[("xb", s), "ident"], w=[pk])
        self.CP("dve", dstT[:, :, t * 128:(t + 1) * 128], pv.rearrange("p (k t) -> p k t", k=8), r=[pk], w=[dkey])

    def proj_fm(self, wbfv, wkey, ct, tb, evac):
        pi = self.psum()
        pk = ("ps", pi)
        for k in range(8):
            self.MM(self.PS[pi][:, :], wbfv[:, k, ct * 128:(ct + 1) * 128], self.xT[:, k, tb * 512:(tb + 1) * 512],
                    k == 0, k == 7, r=[wkey, ("xT", tb)], w=[pk])
        evac(pi, pk)

    def win_pieces(self, w2d, cols):
        pieces = []
        off = 0
        for (c0, n) in cols:
            src = w2d[:, c0:c0 + n].rearrange("(k p) c -> p k c", p=128)
            pieces.append(((lambda v, off=off, n=n: v[:, :, off:off + n]), src))
            off += n
        return pieces, off

    def mem_kv(self, wmkv):
        for g in range(2):
            pieces, n = self.win_pieces(wmkv, [(g * 256, 256)])
            wv, wk = self.wload(pieces, [128, 8, 256])
            for ct in range(2):
                h = g * 2 + ct
                pi = self.psum(); pk = ("ps", pi)
                for k in range(8):
                    self.MM(self.PS[pi][:, 0:N_MEM], wv[:, k, ct * 128:(ct + 1) * 128], self.memT[:, k, :], k == 0, k == 7,
                            r=[wk, ("memT", 0), ("memT", 1)], w=[pk])
                self.CP("act", self.kTm[:, h, :], self.PS[pi][:, 0:N_MEM], r=[pk], w=[("kTm", h)])
        for g in range(2):
            pieces, n = self.win_pieces(wmkv, [(512 + g * 256, 256)])
            wv, wk = self.wload(pieces, [128, 8, 256])
            for c in range(2):
                pi = self.psum(); pk = ("ps", pi)
                for k in range(8):
                    self.MM(self.PS[pi][:, 0:256], self.memT[:, k, c * 128:(c + 1) * 128], wv[:, k, :], k == 0, k == 7,
                            r=[wk, ("memT", c)], w=[pk])
                self.CP("act", self.vm[:, c, g * 256:(g + 1) * 256], self.PS[pi][:, 0:256], r=[pk], w=[("vm", c, g)])

    def _ak(self, slot, scr):
        sm = scr["sm"]
        sid = (scr["id"], slot)
        c0s = 16 + 8 * slot
        cols = [sm[:, c0s + i:c0s + i + 1] for i in range(4)]
        keys = [("smc", sid, i) for i in range(4)]
        return sid, cols, keys

    def at_qk(self, job, slot, scr):
        qT, qkeys, kT, kkeys, nk, bias = job["qT"], job["qkeys"], job["kT"], job["kkeys"], job["nk"], job["bias"]
        sid, cols, keys = self._ak(slot, scr)
        nch = (nk + 511) // 512
        if bias is not None:
            Dsl, ch, M, mkeys = bias
            z = scr["z"][slot]
            zk = ("z", sid)
            for c in range(nch):
                c0 = c * 512
                n = min(512, nk - c0)
                pi = self.psum(); pk = ("ps", pi)
                self.MM(self.PS[pi][:, 0:n], self.ident[:, :], M[:, c0:c0 + n], True, False, r=["ident"] + list(mkeys), w=[pk])
                self.MM(self.PS[pi][:, 0:n], qT, kT[:, c0:c0 + n], False, True, r=list(qkeys) + list(kkeys), w=[pk])
                self.STT("dve", z[:, c0:c0 + n], Dsl[:, c0:c0 + n], ch, self.PS[pi][:, 0:n], ALU.mult, ALU.add,
                         r=[pk, "dtab"], w=[zk])
        else:
            pi = self.psum(); pk = ("ps", pi)
            self.MM(self.PS[pi][:, 0:nk], qT, kT[:, 0:nk], True, True, r=list(qkeys) + list(kkeys), w=[pk])
            job["_ps"] = pi

    def at_max(self, job, slot, scr):
        nk = job["nk"]
        sid, (cmx, cnm, crs, cri), (kmx, knm, krs, kri) = self._ak(slot, scr)
        p = scr["p"][slot]
        pkey = ("p", sid)
        if job["bias"] is not None:
            src, sk = scr["z"][slot][:, 0:nk], ("z", sid)
        else:
            src, sk = self.PS[job["_ps"]][:, 0:nk], ("ps", job["_ps"])
        self.TS("dve", p[:, 0:nk], src, -SCALE, None, ALU.mult, ALU.min, r=[sk], w=[pkey, kmx, knm], accum=cnm)

    def at_exp(self, job, slot, scr):
        nk = job["nk"]
        sid, (cmx, cnm, crs, cri), (kmx, knm, krs, kri) = self._ak(slot, scr)
        p = scr["p"][slot]
        pkey = ("p", sid)
        if job["bias"] is not None:
            src, sk = scr["z"][slot][:, 0:nk], ("z", sid)
        else:
            src, sk = self.PS[job["_ps"]][:, 0:nk], ("ps", job["_ps"])
        self.ACT(p[:, 0:nk], src, AF.Exp, r=[sk, knm], w=[pkey, krs], bias=cnm, scale=SCALE, accum=crs)

    def at_tr(self, job, slot, scr):
        nk = job["nk"]
        sid, cols, keys = self._ak(slot, scr)
        p = scr["p"][slot]
        pkey = ("p", sid)
        nc128 = nk // 128
        banks = []
        for b0 in range(0, nc128, 8):
            nb = min(8, nc128 - b0)
            pi = self.psum(); pk = ("ps", pi)
            pv = self.PS[pi][:].bitcast(BF16)
            for c in range(nb):
                self.TR(pv[:, c * 128:(c + 1) * 128], p[:, (b0 + c) * 128:(b0 + c + 1) * 128], self.ident[:],
                        r=[pkey, "ident"], w=[pk])
            banks.append((b0, nb, pi))
        job["_tb"] = banks

    def at_evac(self, job, slot, scr):
        pT = scr["pT"]
        ptk = ("pT", scr["id"])
        for (b0, nb, pi) in job["_tb"]:
            pv = self.PS[pi][:].bitcast(BF16)
            self.CP("act", pT[:, b0:b0 + nb, :], pv[:, 0:nb * 128].rearrange("p (c t) -> p c t", c=nb), r=[("ps", pi)], w=[(ptk, b0)])

    def at_pv(self, job, slot, scr):
        nk, vfn, vkeys = job["nk"], job["vfn"], job["vkeys"]
        pT = scr["pT"]
        ptk = ("pT", scr["id"])
        nc128 = nk // 128
        pi = self.psum(); pk = ("ps", pi)
        for c in range(nc128):
            self.MM(self.PS[pi][:, 0:128], pT[:, c, :], vfn(c), c == 0, c == nc128 - 1,
                    r=[(ptk, (c // 8) * 8)] + list(vkeys), w=[pk])
        job["_pv"] = pi

    def at_fin(self, job, slot, scr):
        sid, (cmx, cnm, crs, cri), (kmx, knm, krs, kri) = self._ak(slot, scr)
        self.S.op("dve", (lambda e, cri=cri, crs=crs: e.reciprocal(cri, crs)), r=[krs], w=[kri])
        pi = job["_pv"]
        self.TS("dve", job["ydst"], self.PS[pi][:, 0:128], cri, None, ALU.mult, None, r=[("ps", pi), kri], w=[job["ydkey"]])
        if job.get("post") is not None:
            job["post"]()

    def attn_pipeline_gen(self, jobs, scr):
        n = len(jobs)
        for i in range(n + 1):
            cur = jobs[i] if i < n else None
            prv = jobs[i - 1] if i >= 1 else None
            cs, ps_ = i % 2, (i - 1) % 2
            if cur is not None:
                self.at_qk(cur, cs, scr)
            if prv is not None:
                self.at_tr(prv, ps_, scr)
                self.at_evac(prv, ps_, scr)
            if cur is not None:
                self.at_max(cur, cs, scr)
                self.at_exp(cur, cs, scr)
            if prv is not None:
                self.at_pv(prv, ps_, scr)
                self.at_fin(prv, ps_, scr)
            yield

    def attn_pipeline(self, jobs, scr, side=None, per=1):
        for _ in self.attn_pipeline_gen(jobs, scr):
            if side is not None:
                for _k in range(per):
                    next(side, None)

    def ytok_to_QY(self, ytok, ykeys, f0, nf, qt):
        for b0 in range(0, nf, 8):
            nb = min(8, nf - b0)
            pi = self.psum(); pk = ("ps", pi)
            pv = self.PS[pi][:].bitcast(BF16)
            for c in range(nb):
                self.TR(pv[:, c * 128:(c + 1) * 128], ytok[:, (b0 + c) * 128:(b0 + c + 1) * 128], self.ident[:],
                        r=list(ykeys) + ["ident"], w=[pk])
            self.CP("act", self.QY[:, f0 + b0:f0 + b0 + nb, qt * 128:(qt + 1) * 128],
                    pv[:, 0:nb * 128].rearrange("p (c t) -> p c t", c=nb), r=[pk],
                    w=[("QY", f0 + b0 + c, qt) for c in range(nb)])

    def mem_attention(self, scr, ytok):
        jobs = []
        for qt in range(16):
            for h in range(4):
                job = dict(qT=self.QY[:, 12 + h, qt * 128:(qt + 1) * 128], qkeys=[("QY", 12 + h, qt)],
                           kT=self.kTm[:, h, :], kkeys=[("kTm", h)], nk=N_MEM,
                           vfn=(lambda c, h=h: self.vm[:, c, h * 128:(h + 1) * 128]),
                           vkeys=[("vm", 0, 0), ("vm", 0, 1), ("vm", 1, 0), ("vm", 1, 1)],
                           bias=None, ydst=ytok[:, h * 128:(h + 1) * 128], ydkey=("ytok", h), post=None)
                if h == 3:
                    job["post"] = (lambda qt=qt: self.ytok_to_QY(ytok, [("ytok", hh) for hh in range(4)], 12, 4, qt))
                jobs.append(job)
        return self.attn_pipeline_gen(jobs, scr)

    def layer_A(self, j):
        nc = self.nc
        S = self.S
        w_in = self.a_w_in[j]
        with ExitStack() as st:
            sb = lambda name, shape, dtype: st.enter_context(nc.sbuf_tensor(self.nm(name), shape, dtype))
            hbuf = [sb("h%d" % i, [128, 2080], BF16) for i in range(2)]
            dg = [sb("dg%d" % i, [128, CONV_K, 128], BF16) for i in range(2)]
            sig = [sb("sig%d" % i, [128, 512], F32) for i in range(2)]
            spA = sb("spA", [128, 36 + 12 * CONV_K], F32)
            stt = [sb("stt%d" % i, [128, 512], F32) for i in range(4)]
            sq = [sb("sq%d" % i, [128, 512], BF16) for i in range(2)]
            scr = {"id": "A", "p": [sb("pA%d" % i, [128, 256], BF16) for i in range(2)],
                   "pT": sb("pTA", [128, 2, 128], BF16), "sm": self.sm}
            ytok = sb("ytokA", [128, 512], BF16)

            self.DMA(spA[:], self.a_sp[j], "spA", r=[], w=["spA"])
            for i in range(2):
                self.MEMSET("dve", hbuf[i][:, 0:32], 0.0, w=[("h", i)])
            self.mem_kv(self.a_w_mkv[j])

            for g in range(2):
                pieces, n = self.win_pieces(w_in, [(3072 + g * 256, 256)])
                wv, wk = self.wload(pieces, [128, 8, 256])
                for ct in range(2):
                    f = 12 + g * 2 + ct
                    for tb in range(4):
                        def ev(pi, pk, f=f, tb=tb):
                            self.CP("act", self.QY[:, f, tb * 512:(tb + 1) * 512], self.PS[pi][:, :], r=[pk],
                                    w=[("QY", f, tb * 4 + q) for q in range(4)])
                        self.proj_fm(wv, wk, ct, tb, ev)

            mgen = self.mem_attention(scr, ytok)
            for c in range(12):
                hs = c % 2
                hk = ("h", hs)
                pieces, n = self.win_pieces(w_in, [(c * 128, 128), (1536 + c * 128, 128)])
                wv, wk = self.wload(pieces, [128, 8, 256])
                self.TT("dve", dg[hs][:, :, :],
                        self.identf[:, :].unsqueeze(1).to_broadcast([128, CONV_K, 128]),
                        spA[:, 36 + c * CONV_K:36 + (c + 1) * CONV_K].unsqueeze(2).to_broadcast([128, CONV_K, 128]),
                        ALU.mult, r=["identf", "spA"], w=[("dg", hs)])
                for tb in range(4):
                    ss = tb % 2
                    def ev_g(pi, pk, ss=ss):
                        self.ACT(sig[ss][:, :], self.PS[pi][:, :], AF.Sigmoid, r=[pk], w=[("sig", ss)])
                    def ev_a(pi, pk, ss=ss, tb=tb, hs=hs, hk=hk):
                        self.TT("dve", hbuf[hs][:, 30 + tb * 512:30 + (tb + 1) * 512], self.PS[pi][:, :], sig[ss][:, :],
                                ALU.mult, r=[pk, ("sig", ss)], w=[(hk, tb)])
                    self.proj_fm(wv, wk, 1, tb, ev_g)
                    self.proj_fm(wv, wk, 0, tb, ev_a)
                for tb in range(4):
                    pi = self.psum(); pk = ("ps", pi)
                    rk = [hk, ("dg", hs)] + [(hk, t2) for t2 in range(max(0, tb - 1), tb + 1)]
                    for k in range(CONV_K):
                        self.MM(self.PS[pi][:, :], dg[hs][:, k, :], hbuf[hs][:, tb * 512 + k:tb * 512 + k + 512],
                                k == 0, k == CONV_K - 1, r=rk, w=[pk])
                    self.ACT(self.QY[:, c, tb * 512:(tb + 1) * 512], self.PS[pi][:, :], AF.Identity, r=[pk, "spA"],
                             w=[("QY", c, tb * 4 + q) for q in range(4)], bias=spA[:, c:c + 1], scale=1.0)
            for _ in mgen:
                pass

            for tb in range(4):
                tsl = slice(tb * 512, (tb + 1) * 512)
                p1 = self.psum(); p2 = self.psum()
                for c in range(12):
                    qk = [("QY", c, tb * 4 + q) for q in range(4)]
                    ss = c % 2
                    self.ACT(sq[ss][:, :], self.QY[:, c, tsl], AF.Square, r=qk, w=[("sq", ss)])
                    self.MM(self.PS[p1][:, :], self.ones[:, :], self.QY[:, c, tsl], c == 0, c == 11, r=qk + ["ones"], w=[("ps", p1)])
                    self.MM(self.PS[p2][:, :], self.ones[:, :], sq[ss][:, :], c == 0, c == 11, r=[("sq", ss), "ones"], w=[("ps", p2)])
                mean, msq, var, rstd = stt
                self.ACT(mean[:, :], self.PS[p1][:, :], AF.Identity, r=[("ps", p1)], w=["stt0"], scale=1.0 / CONV_W)
                self.TT("dve", msq[:, :], mean[:, :], mean[:, :], ALU.mult, r=["stt0"], w=["stt1"])
                self.STT("dve", var[:, :], self.PS[p2][:, :], 1.0 / CONV_W, msq[:, :], ALU.mult, ALU.subtract,
                         r=[("ps", p2), "stt1"], w=["stt2"])
                self.TS("dve", var[:, :], var[:, :], LN_EPS, None, ALU.add, None, r=["stt2"], w=["stt2"])
                self.S.op("act", lambda e, var=var: e.sqrt(var[:, :], var[:, :]), r=["stt2"], w=["stt2"])
                self.S.op("dve", lambda e, var=var, rstd=rstd: e.reciprocal(rstd[:, :], var[:, :]), r=["stt2"], w=["stt3"])
                for c in range(12):
                    qk = [("QY", c, tb * 4 + q) for q in range(4)]
                    ss = c % 2
                    self.TT("dve", sig[ss][:, :], self.QY[:, c, tsl], mean[:, :], ALU.subtract, r=qk + ["stt0"], w=[("sig", ss)])
                    self.TT("dve", sig[ss][:, :], sig[ss][:, :], rstd[:, :], ALU.mult, r=[("sig", ss), "stt3"], w=[("sig", ss)])
                    self.ACT(self.QY[:, c, tsl], sig[ss][:, :], AF.Silu, r=[("sig", ss), "spA"], w=qk,
                             bias=spA[:, 24 + c:25 + c], scale=spA[:, 12 + c:13 + c])

    def layer_B(self, j):
        nc = self.nc
        S = self.S
        w_in = self.b_w_in[j]
        slopes = _alibi_slopes(ATT_H)
        with ExitStack() as st:
            sb = lambda name, shape, dtype: st.enter_context(nc.sbuf_tensor(self.nm(name), shape, dtype))
            kT = sb("kT", [128, SEQ], BF16)
            v = sb("v", [128, 16, 128], BF16)
            qiT = sb("qiT", [128, 4, SEQ], BF16)
            kiT2 = sb("kiT2", [128, SEQ], BF16)
            wi = sb("wi", [128, 16, 8], F32)
            dtab = sb("dtab", [128, SEQ], F32)
            madm = sb("madm", [128, 256], F32)
            zz = [sb("zB%d" % i, [128, SEQ], F32) for i in range(2)]
            z = zz[0]
            M = sb("MB", [128, SEQ], BF16)
            pp = [sb("pB%d" % i, [128, SEQ], BF16) for i in range(2)]
            junk = pp[0]
            pT = sb("pTB", [128, 16, 128], BF16)
            ytok = sb("ytokB", [128, CONV_W], BF16)
            rl = [sb("rl%d" % i, [128, 512], F32) for i in range(2)]
            sm = self.sm
            scr = {"id": "B", "z": zz, "p": pp, "pT": pT, "sm": sm}
            zk = ("z", ("B", 0))
            jk = ("p", ("B", 0))

            self.DMA(dtab[:], self.c_dtab[:, :], "cst", r=[], w=["dtab"])
            self.DMA(madm[:], self.c_madm[:, :], "cst", r=[], w=["madm"])

            def fm_group(cols, dests, side=None):
                pieces, n = self.win_pieces(w_in, cols)
                wv, wk = self.wload(pieces, [128, 8, n])
                for ct, (dst_fn, key_fn) in enumerate(dests):
                    for tb in range(4):
                        def ev(pi, pk, dst_fn=dst_fn, key_fn=key_fn, tb=tb):
                            self.CP("act", dst_fn(tb), self.PS[pi][:, :], r=[pk], w=key_fn(tb))
                        self.proj_fm(wv, wk, ct, tb, ev)
                        if side is not None:
                            next(side, None)
                            next(side, None)

            def qy_dst(f):
                return ((lambda tb: self.QY[:, f, tb * 512:(tb + 1) * 512]),
                        (lambda tb: [("QY", f, tb * 4 + q) for q in range(4)]))

            fm_group([(1536, 128), (2304, 64), (2304, 64)],
                     [((lambda tb: kT[:, tb * 512:(tb + 1) * 512]), (lambda tb: [("kT", tb)])),
                      ((lambda tb: kiT2[:, tb * 512:(tb + 1) * 512]), (lambda tb: [("kiT2", tb)]))])
            pieces, n = self.win_pieces(w_in, [(1664, 128), (2368, 8)])
            wv, wk = self.wload(pieces, [128, 8, 136])
            for t in range(16):
                pi = self.psum(); pk = ("ps", pi)
                for k in range(8):
                    self.MM(self.PS[pi][:, 0:136], self.xT[:, k, t * 128:(t + 1) * 128], wv[:, k, :], k == 0, k == 7,
                            r=[wk, ("xT", t // 4)], w=[pk])
                self.CP("act", v[:, t, :], self.PS[pi][:, 0:128], r=[pk], w=[("v", t)])
                self.CP("act", wi[:, t, :], self.PS[pi][:, 128:136], r=[pk], w=[("wi", t)])
            for g in range(2):
                fm_group([(1792 + g * 256, 256)],
                         [((lambda tb, f=g * 2 + c: qiT[:, f, tb * 512:(tb + 1) * 512]),
                           (lambda tb, f=g * 2 + c: [("qiT", f, tb)])) for c in range(2)])
            for g in range(2):
                fm_group([(2376 + g * 256, 256)], [qy_dst(12 + g * 2), qy_dst(12 + g * 2 + 1)])
            self.mem_kv(self.b_w_mkv[j])
            mgen = self.mem_attention(scr, ytok)
            for g in range(6):
                fm_group([(g * 256, 256)], [qy_dst(g * 2), qy_dst(g * 2 + 1)])
            for _ in mgen:
                pass

            cA, cLO, cW, cMID, cCNT, cG = 8, 9, 10, 11, 12, 13
            col = lambda c: sm[:, c:c + 1]
            ck = lambda c: ("smb", c)
            isc = self.wst[1]
            isck = ("wst", 1)
            junk = self.wbf[0]
            jk = ("wbf", 0)
            Mb = [M, self.wbf[1]]
            Mk = [[("M", 0)], [("M", 1), ("wbf", 1)]]

            def mask_gen(qt):
                nk = 128 * (qt + 1)
                nch = (nk + 511) // 512
                Mq = Mb[qt % 2]
                mk = Mk[qt % 2]
                if qt < 2:
                    if nk > 128:
                        self.MEMSET("dve", Mq[:, 0:nk - 128], 0.0, w=mk)
                    self.CP("dve", Mq[:, nk - 128:nk], madm[:, 0:128], r=["madm"], w=mk)
                    yield
                    return
                for h in range(IDX_H):
                    hb = (h % 2) * 64
                    for c in range(nch):
                        c0 = c * 512
                        n = min(512, nk - c0)
                        pi = self.psum(); pk = ("ps", pi)
                        ss = (h * nch + c) % 2
                        self.MM(self.PS[pi][:, 0:n], qiT[hb:hb + 64, h // 2, qt * 128:(qt + 1) * 128], kiT2[hb:hb + 64, c0:c0 + n],
                                True, True, r=[("qiT", h // 2, qt // 4), ("kiT2", c)], w=[pk])
                        self.ACT(rl[ss][:, 0:n], self.PS[pi][:, 0:n], AF.Relu, r=[pk], w=[("rl", ss)])
                        if h == 0:
                            self.TS("dve", isc[:, c0:c0 + n], rl[ss][:, 0:n], wi[:, qt, 0:1], None, ALU.mult, None,
                                    r=[("rl", ss), ("wi", qt)], w=[isck])
                        else:
                            self.STT("dve", isc[:, c0:c0 + n], rl[ss][:, 0:n], wi[:, qt, h:h + 1], isc[:, c0:c0 + n], ALU.mult, ALU.add,
                                     r=[("rl", ss), ("wi", qt), isck], w=[isck])
                        yield
                self.TS("dve", junk[:, 0:nk], isc[:, 0:nk], 0.0, None, ALU.add, ALU.max, r=[isck], w=[jk, ck(cA)], accum=col(cA))
                self.TS("dve", junk[:, 0:nk], isc[:, 0:nk], 0.0, None, ALU.add, ALU.min, r=[isck], w=[jk, ck(cLO)], accum=col(cLO))
                yield
                self.TT("dve", col(cW), col(cA), col(cLO), ALU.subtract, r=[ck(cA), ck(cLO)], w=[ck(cW)])
                self.TS("dve", col(cW), col(cW), 1.000002, 1e-20, ALU.mult, ALU.add, r=[ck(cW)], w=[ck(cW)])
                self.TT("dve", isc[:, nk - 128:nk], isc[:, nk - 128:nk], madm[:, 128:256], ALU.add, r=[isck, "madm"], w=[isck])
                yield
                for it in range(NBISECT):
                    cn = 2.0 ** (-(it + 1))
                    self.STT("dve", col(cMID), col(cW), cn, col(cLO), ALU.mult, ALU.add, r=[ck(cW), ck(cLO)], w=[ck(cMID)])
                    self.TS("dve", junk[:, 0:nk], isc[:, 0:nk], col(cMID), None, ALU.is_ge, ALU.add, r=[isck, ck(cMID)],
                            w=[jk, ck(cCNT)], accum=col(cCNT))
                    self.TS("dve", col(cG), col(cCNT), float(TOPK) - 0.5, cn, ALU.is_ge, ALU.mult, r=[ck(cCNT)], w=[ck(cG)])
                    self.STT("dve", col(cLO), col(cG), col(cW), col(cLO), ALU.mult, ALU.add, r=[ck(cG), ck(cW), ck(cLO)], w=[ck(cLO)])
                    yield
                self.TS("dve", Mq[:, 0:nk], isc[:, 0:nk], col(cLO), MASKNEG, ALU.is_lt, ALU.mult, r=[isck, ck(cLO)], w=mk)
                yield

            for _ in mask_gen(0):
                pass
            for qt in range(16):
                nk = 128 * (qt + 1)
                nch = (nk + 511) // 512
                d0 = 1920 - 128 * qt
                jobs = []
                for h in range(ATT_H):
                    jobs.append(dict(qT=self.QY[:, h, qt * 128:(qt + 1) * 128], qkeys=[("QY", h, qt)],
                                     kT=kT[:, 0:nk], kkeys=[("kT", c) for c in range(nch)], nk=nk,
                                     vfn=(lambda c: v[:, c, :]), vkeys=[("v", c) for c in range(nk // 128)],
                                     bias=(dtab[:, d0:d0 + nk], -slopes[h] / SCALE, Mb[qt % 2], [("M", qt % 2)]),
                                     ydst=ytok[:, h * 128:(h + 1) * 128], ydkey=("ytok", h), post=None))
                side = mask_gen(qt + 1) if qt < 15 else None
                self.attn_pipeline(jobs, scr, side=side, per=5)
                if side is not None:
                    for _ in side:
                        pass
                self.ytok_to_QY(ytok, [("ytok", h) for h in range(ATT_H)], 0, ATT_H, qt)

    def gate_out(self, wgate, goff, wout, pgbsrc, xsrc, xdst, last, li):
        nc = self.nc
        S = self.S
        with ExitStack() as st:
            sb = lambda name, shape, dtype: st.enter_context(nc.sbuf_tensor(self.nm(name), shape, dtype))
            woutb = sb("woutb", [128, 16, D_MODEL], BF16)
            self.xio = [sb("xio%d" % i, [128, D_MODEL], F32) for i in range(2)]
            self.xb = [sb("xb%d" % i, [128, D_MODEL], BF16) for i in range(2)]
            self.pgb = sb("pgb", [128, 2 * D_MODEL], F32)
            self.lnst = sb("lnst", [128, 12], F32)
            self.lnag = sb("lnag", [128, 4], F32)
            gt = [sb("gt%d" % i, [128, 512], BF16) for i in range(2)]
            self.DMA(self.pgb[:], pgbsrc, "pgb", r=[], w=["pgb"])
            for g in range(8):
                pieces, n = self.win_pieces(wgate, [(goff + g * 256, 256)])
                wv, wk = self.wload(pieces, [128, 8, 256])
                for ct in range(2):
                    f = g * 2 + ct
                    for tb in range(4):
                        def ev(pi, pk, f=f, tb=tb):
                            ss = tb % 2
                            qk = [("QY", f, tb * 4 + q) for q in range(4)]
                            self.ACT(gt[ss][:, :], self.PS[pi][:, :], AF.Silu, r=[pk], w=[("gt", ss)])
                            self.TT("dve", self.QY[:, f, tb * 512:(tb + 1) * 512], self.QY[:, f, tb * 512:(tb + 1) * 512],
                                    gt[ss][:, :], ALU.mult, r=qk + [("gt", ss)], w=qk)
                        self.proj_fm(wv, wk, ct, tb, ev)
                s = self.w_rr % 2
                self.w_rr += 1
                stv = self.wst[s][:, :].rearrange("p (a b) -> p a b", a=2)
                self.DMA(stv, wout[g * 256:(g + 1) * 256, :].rearrange("(k p) c -> p k c", p=128), "wst%d" % s, r=[], w=[("wst", s)])
                self.CP("pool", woutb[:, 2 * g:2 * g + 2, :], stv, r=[("wst", s)], w=[("wout", g)])
            for t in range(16):
                s = t % 2
                xk = ("xio", s)
                self.DMA(self.xio[s][:], xsrc[t * 128:(t + 1) * 128, :], "xio%d" % s, r=[("xdram", li - 1, t)], w=[xk])
                for half in range(2):
                    pi = self.psum(); pk = ("ps", pi)
                    for f in range(16):
                        self.MM(self.PS[pi][:, :], self.QY[:, f, t * 128:(t + 1) * 128], woutb[:, f, half * 512:(half + 1) * 512],
                                f == 0, f == 15, r=[("QY", f, t), ("wout", f // 2)], w=[pk])
                    self.STT("dve", self.xio[s][:, half * 512:(half + 1) * 512], self.xio[s][:, half * 512:(half + 1) * 512], ALPHA,
                             self.PS[pi][:, :], ALU.mult, ALU.add, r=[pk, xk], w=[xk])
                self.layernorm_tile(s)
                self.DMA(xdst[t * 128:(t + 1) * 128, :], self.xio[s][:], "xout%d" % s, r=[xk], w=[("xdram", li, t)])
                if not last:
                    self.to_T(s, self.xT, t, ("xT", t // 4))


_PROG_CACHE = {}


def _get_prog(layers):
    key = tuple(layers)
    if key not in _PROG_CACHE:
        p = Prog(layers)
        p.build()
        _PROG_CACHE[key] = p
    return _PROG_CACHE[key]


def _consts():
    ident = np.eye(128, dtype=np.float32)
    p = np.arange(128, dtype=np.float32)[:, None]
    jj = np.arange(SEQ, dtype=np.float32)[None, :]
    dtab = np.abs(p - (jj - 1920.0)).astype(np.float32)
    madm = np.zeros((128, 256), dtype=np.float32)
    madm[0:64, 64:128] = MASKNEG
    madm[0:64, 128 + 64:256] = -1e30
    return ident, dtab, madm


def _host_inputs(inp, b):
    f = lambda a: np.ascontiguousarray(np.asarray(a, dtype=np.float32))
    ident, dtab, madm = _consts()
    rep = lambda v: np.ascontiguousarray(np.broadcast_to(np.asarray(v, np.float32)[None, :], (128, v.shape[-1])))
    a_sp = np.zeros((2, 128, 36 + 12 * CONV_K), np.float32)
    for j in range(2):
        a_sp[j, :, 0:12] = np.asarray(inp["a_conv_b"][j]).reshape(12, 128).T
        a_sp[j, :, 12:24] = np.asarray(inp["a_ln_g"][j]).reshape(12, 128).T
        a_sp[j, :, 24:36] = np.asarray(inp["a_ln_b"][j]).reshape(12, 128).T
        cw = np.asarray(inp["a_conv_w"][j]).reshape(CONV_K, 12, 128)
        a_sp[j, :, 36:] = np.transpose(cw, (2, 1, 0)).reshape(128, 12 * CONV_K)
    a_pgb = np.stack([np.concatenate([rep(inp["a_post_g"][j]), rep(inp["a_post_b"][j])], axis=1) for j in range(2)])
    b_pgb = np.stack([np.concatenate([rep(inp["b_post_g"][j]), rep(inp["b_post_b"][j])], axis=1) for j in range(2)])
    memgb = np.concatenate([rep(np.asarray(inp["mem_ln_g"])), rep(np.asarray(inp["mem_ln_b"]))], axis=1)
    return {
        "x": f(inp["x"][b]), "mem": f(inp["mem"][b]), "memgb": f(memgb),
        "c_ident": ident, "c_dtab": dtab, "c_madm": madm,
        "a_w_in": f(inp["a_w_in"]), "a_sp": f(a_sp), "a_w_mkv": f(inp["a_w_mkv"]), "a_w_out": f(inp["a_w_out"]),
        "a_pgb": f(a_pgb),
        "b_w_in": f(inp["b_w_in"]), "b_w_mkv": f(inp["b_w_mkv"]), "b_w_out": f(inp["b_w_out"]), "b_pgb": f(b_pgb),
    }


def run_layers(inp, layers, cores):
    prog = _get_prog(layers)
    maps = [_host_inputs(inp, b) for b in cores]
    shared = {}
    for m in maps[1:]:
        for k in m:
            if k not in ("x", "mem"):
                m[k] = maps[0][k]
    res = run_bass_kernel_spmd(prog.nc, maps, core_ids=list(range(len(cores))))
    if DEBUG:
        global _DBG
        _DBG = [{k: np.asarray(r[k]) for k in ("dbg", "dbg_k", "dbg_v", "dbg_m")} for r in res.results]
    return np.stack([r["out"] for r in res.results], axis=0)


def kernel(**inputs):
    inp = {k: np.asarray(v) for k, v in inputs.items()}
    out = run_layers(inp, [0, 1, 2, 3], list(range(8)))
    return out.astype(np.float32)
```

```python
import numpy as np
from contextlib import ExitStack
import concourse.bass as bass
import concourse.mybir as mybir
from concourse.bass_utils import run_bass_kernel_spmd

F32 = mybir.dt.float32
BF16 = mybir.dt.bfloat16
AF = mybir.ActivationFunctionType
ALU = mybir.AluOpType
AX = mybir.AxisListType

D_MODEL = 1024
SEQ = 2048
DEPTH = 4
N_MEM = 256
HD = 128
CONV_W = 1536
CONV_K = 31
ATT_H = 12
IDX_H = 8
IDX_D = 64
TOPK = 256
ALPHA = (2 * DEPTH) ** 0.25
LN_EPS = 1e-5
SCALE = HD ** -0.5
A_IN = 5632
B_IN = 4936
NBISECT = 20
DEBUG = False
MASKNEG = -30000.0


def _alibi_slopes(n):
    import math
    p = 2 ** int(math.floor(math.log2(n)))
    base = [2.0 ** (-8.0 * (i + 1) / p) for i in range(p)]
    extra = [2.0 ** (-4.0 * (2 * i + 1) / p) for i in range(n - p)]
    return base + extra


class _Op:
    __slots__ = ("eng", "fn", "deps", "dma", "sig", "cnt", "waits", "clock", "gid")


class Sched:
    ENG = ("pe", "act", "dve", "pool", "sp")

    def __init__(self):
        self.ops = []
        self.lastw = {}
        self.readers = {}
        self.streams = {}
        self.last_on = {}
        self.pending_dma = []

    def op(self, eng, fn, r=(), w=(), dma=None, extra=()):
        o = _Op()
        o.eng = eng; o.fn = fn; o.dma = dma; o.sig = False; o.gid = len(self.ops)
        o.cnt = 0; o.clock = None; o.waits = ()
        deps = {}

        def add(d, kind):
            if d is None or d is o:
                return
            if d.dma is None and dma is None and d.eng == eng:
                if eng == "pe":
                    return
                if kind == "war":
                    return
            deps[d.gid] = d

        for k in r:
            add(self.lastw.get(k), "raw")
            if isinstance(k, tuple) and k[0] == "ps":
                rd = self.readers.get(k)
                if rd:
                    for x in rd.values():
                        if x.eng != eng:
                            add(x, "raw")
        for k in w:
            add(self.lastw.get(k), "waw")
            rd = self.readers.get(k)
            if rd:
                for x in rd.values():
                    add(x, "war")
        for d in extra:
            add(d, "raw")
        for k in w:
            self.lastw[k] = o
            self.readers[k] = {}
        for k in r:
            rd = self.readers.setdefault(k, {})
            rd[("dma", o.gid) if dma is not None else eng] = o
        o.deps = list(deps.values())
        self.ops.append(o)
        self.last_on[eng] = o
        if dma is not None:
            self.streams[dma] = self.streams.get(dma, 0) + 1
            o.cnt = self.streams[dma] * 16
            self.pending_dma.append(o)
        return o

    def barrier(self):
        last = [self.last_on[e] for e in self.ENG if e in self.last_on]
        dmas = list(self.pending_dma)
        self.pending_dma = []
        for e in self.ENG:
            ex = [d for d in last if d.eng != e or d.dma is not None] + dmas
            self.op(e, (lambda en: en.nop()), extra=ex)

    def finalize(self):
        for o in self.ops:
            for d in o.deps:
                d.sig = True
        cnt = {e: 0 for e in self.ENG}
        for o in self.ops:
            if o.dma is None and o.sig:
                cnt[o.eng] += 1
                o.cnt = cnt[o.eng]
        eclock = {e: {} for e in self.ENG}
        for o in self.ops:
            ck = eclock[o.eng]
            waits = {}
            for d in sorted(o.deps, key=lambda t: t.gid):
                key = d.dma if d.dma is not None else d.eng
                if ck.get(key, 0) >= d.cnt:
                    continue
                waits[key] = max(waits.get(key, 0), d.cnt)
                for k2, v2 in d.clock.items():
                    if ck.get(k2, 0) < v2:
                        ck[k2] = v2
            o.waits = tuple(waits.items())
            if o.dma is not None:
                c2 = dict(ck); c2[o.dma] = max(c2.get(o.dma, 0), o.cnt); o.clock = c2
            elif o.sig:
                c2 = dict(ck); c2[o.eng] = o.cnt; o.clock = c2
        self.byeng = {e: [o for o in self.ops if o.eng == e] for e in self.ENG}

    def emit(self, nc, stack):
        self.finalize()
        sems = {}
        for key in list(self.ENG) + list(self.streams.keys()):
            sems[key] = stack.enter_context(nc.semaphore("s_" + str(key)))
        streams = self.streams
        byeng = self.byeng

        def runner(engname):
            def f(e):
                for o in byeng[engname]:
                    for key, val in o.waits:
                        e.wait_ge(sems[key], val)
                    ins = o.fn(e)
                    if o.dma is not None:
                        ins.then_inc(sems[o.dma], 16)
                    elif o.sig:
                        ins.then_inc(sems[o.eng], 1)
                if engname == "sp":
                    for key, n in streams.items():
                        e.wait_ge(sems[key], 16 * n)
            return f

        with nc.Block() as block:
            block.tensor(runner("pe"))
            block.scalar(runner("act"))
            block.vector(runner("dve"))
            block.gpsimd(runner("pool"))
            block.sync(runner("sp"))


class Prog:
    def __init__(self, layers, final_to_out=True):
        self.layers = list(layers)
        self.S = Sched()
        self.nc = bass.Bass("TRN2", target_bir_lowering=False)
        self.ps_rr = 0
        self.uid = 0

    def nm(self, name):
        self.ncnt = getattr(self, "ncnt", 0) + 1
        return "%s_%d" % (name, self.ncnt)

    def MM(self, out, lhsT, rhs, start, stop, r, w):
        return self.S.op("pe", lambda e: e.matmul(out, lhsT, rhs, start=start, stop=stop), r=r, w=w)

    def TR(self, out, in_, ident, r, w):
        return self.S.op("pe", lambda e: e.transpose(out, in_, ident), r=r, w=w)

    def ACT(self, out, in_, func, r, w, bias=None, scale=None, accum=None):
        kw = {}
        if bias is not None:
            kw["bias"] = bias
        if scale is not None:
            kw["scale"] = scale
        if accum is not None:
            kw["accum_out"] = accum
        return self.S.op("act", lambda e: e.activation(out=out, in_=in_, func=func, **kw), r=r, w=w)

    def TS(self, eng, out, in0, s1, s2, op0, op1, r, w, accum=None):
        kw = {}
        if op1 is not None:
            kw["op1"] = op1
        if accum is not None:
            kw["accum_out"] = accum
        return self.S.op(eng, lambda e: e.tensor_scalar(out=out, in0=in0, scalar1=s1, scalar2=s2, op0=op0, **kw), r=r, w=w)

    def TT(self, eng, out, in0, in1, op, r, w):
        return self.S.op(eng, lambda e: e.tensor_tensor(out=out, in0=in0, in1=in1, op=op), r=r, w=w)

    def STT(self, eng, out, in0, scalar, in1, op0, op1, r, w):
        return self.S.op(eng, lambda e: e.scalar_tensor_tensor(out=out, in0=in0, scalar=scalar, in1=in1, op0=op0, op1=op1), r=r, w=w)

    def CP(self, eng, out, in_, r, w):
        if eng == "act":
            return self.S.op("act", lambda e: e.copy(out=out, in_=in_), r=r, w=w)
        return self.S.op(eng, lambda e: e.tensor_copy(out=out, in_=in_), r=r, w=w)

    def MEMSET(self, eng, ap, val, w):
        return self.S.op(eng, lambda e: e.memset(ap, val), w=w)

    def DMA(self, out, in_, stream, r, w):
        return self.S.op("sp", lambda e: e.dma_start(out=out, in_=in_), r=r, w=w, dma=stream)

    def psum(self):
        i = self.ps_rr
        self.ps_rr = (self.ps_rr + 1) % 8
        return i

    def wload(self, pieces, shape):
        s = self.w_rr % 2
        b = self.wb_rr % 2
        self.w_rr += 1
        self.wb_rr += 1
        a, bb = shape[1], shape[2]
        n = a * bb
        assert n <= 2048
        stv = self.wst[s][:, 0:n].rearrange("p (a b) -> p a b", a=a)
        bfv = self.wbf[b][:, 0:n].rearrange("p (a b) -> p a b", a=a)
        for dst_fn, src in pieces:
            self.DMA(dst_fn(stv), src, "wst%d" % s, r=[], w=[("wst", s)])
        self.CP("pool", bfv, stv, r=[("wst", s)], w=[("wbf", b)])
        return bfv, ("wbf", b)

    def build(self):
        nc = self.nc
        S = self.S
        L = self.layers
        dt = nc.dram_tensor
        self.x_in = dt("x", [SEQ, D_MODEL], F32, kind="ExternalInput").ap()
        self.mem_in = dt("mem", [N_MEM, D_MODEL], F32, kind="ExternalInput").ap()
        self.memgb = dt("memgb", [128, 2 * D_MODEL], F32, kind="ExternalInput").ap()
        self.c_ident = dt("c_ident", [128, 128], F32, kind="ExternalInput").ap()
        self.c_dtab = dt("c_dtab", [128, SEQ], F32, kind="ExternalInput").ap()
        self.c_madm = dt("c_madm", [128, 256], F32, kind="ExternalInput").ap()
        self.a_w_in = dt("a_w_in", [2, D_MODEL, A_IN], F32, kind="ExternalInput").ap()
        self.a_sp = dt("a_sp", [2, 128, 36 + 12 * CONV_K], F32, kind="ExternalInput").ap()
        self.a_w_mkv = dt("a_w_mkv", [2, D_MODEL, 1024], F32, kind="ExternalInput").ap()
        self.a_w_out = dt("a_w_out", [2, 2048, D_MODEL], F32, kind="ExternalInput").ap()
        self.a_pgb = dt("a_pgb", [2, 128, 2 * D_MODEL], F32, kind="ExternalInput").ap()
        self.b_w_in = dt("b_w_in", [2, D_MODEL, B_IN], F32, kind="ExternalInput").ap()
        self.b_w_mkv = dt("b_w_mkv", [2, D_MODEL, 1024], F32, kind="ExternalInput").ap()
        self.b_w_out = dt("b_w_out", [2, 2048, D_MODEL], F32, kind="ExternalInput").ap()
        self.b_pgb = dt("b_pgb", [2, 128, 2 * D_MODEL], F32, kind="ExternalInput").ap()
        self.out = dt("out", [SEQ, D_MODEL], F32, kind="ExternalOutput").ap()
        self.dbg = dt("dbg", [128, 16 * SEQ], BF16, kind="ExternalOutput").ap() if DEBUG else None
        if DEBUG:
            self.dbg_k = dt("dbg_k", [128, 4 * 256], BF16, kind="ExternalOutput").ap()
            self.dbg_v = dt("dbg_v", [128, 2 * 512], BF16, kind="ExternalOutput").ap()
            self.dbg_m = dt("dbg_m", [128, 8 * 256], BF16, kind="ExternalOutput").ap()
        self.xs = [dt("xs0", [SEQ, D_MODEL], F32).ap(), dt("xs1", [SEQ, D_MODEL], F32).ap()]

        with ExitStack() as st:
            sb = lambda name, shape, dtype: st.enter_context(nc.sbuf_tensor(self.nm(name), shape, dtype))
            self.PS = [st.enter_context(nc.psum_tensor("ps%d" % i, [128, 512], F32)) for i in range(8)]
            self.identf = sb("identf", [128, 128], F32)
            self.ident = sb("ident", [128, 128], BF16)
            self.ones = sb("ones", [128, 128], BF16)
            self.memT = sb("memT", [128, 8, N_MEM], BF16)
            self.xT = sb("xT", [128, 8, SEQ], BF16)
            self.QY = sb("QY", [128, 16, SEQ], BF16)
            self.wst = [sb("wst%d" % i, [128, 2048], F32) for i in range(2)]
            self.wbf = [sb("wbf%d" % i, [128, 2048], BF16) for i in range(2)]
            self.kTm = sb("kTm", [128, 4, N_MEM], BF16)
            self.vm = sb("vm", [128, 2, 512], BF16)
            self.sm = sb("sm", [128, 64], F32)
            self.w_rr = 0
            self.wb_rr = 0

            self.DMA(self.identf[:], self.c_ident[:, :], "cst", r=[], w=["identf"])
            self.CP("dve", self.ident[:], self.identf[:], r=["identf"], w=["ident"])
            self.MEMSET("dve", self.ones[:], 1.0, w=["ones"])

            with ExitStack() as st0:
                sb0 = lambda name, shape, dtype: st0.enter_context(nc.sbuf_tensor(self.nm(name), shape, dtype))
                self.xio = [sb0("xio%d" % i, [128, D_MODEL], F32) for i in range(2)]
                self.xb = [sb0("xb%d" % i, [128, D_MODEL], BF16) for i in range(2)]
                self.pgb = sb0("pgb", [128, 2 * D_MODEL], F32)
                self.lnst = sb0("lnst", [128, 12], F32)
                self.lnag = sb0("lnag", [128, 4], F32)
                self.DMA(self.pgb[:], self.memgb[:, :], "pgb", r=[], w=["pgb"])
                for t in range(2):
                    s = t % 2
                    self.DMA(self.xio[s][:], self.mem_in[t * 128:(t + 1) * 128, :], "xio%d" % s, r=[], w=[("xio", s)])
                    self.layernorm_tile(s)
                    self.to_T(s, self.memT, t, ("memT", t))
                for t in range(16):
                    s = t % 2
                    self.DMA(self.xio[s][:], self.x_in[t * 128:(t + 1) * 128, :], "xio%d" % s, r=[], w=[("xio", s)])
                    self.CP("act", self.xb[s][:], self.xio[s][:], r=[("xio", s)], w=[("xb", s)])
                    self.to_T(s, self.xT, t, ("xT", t // 4))
            S.barrier()

            xsrc = self.x_in
            for li, layer in enumerate(L):
                j = layer // 2
                last = (li == len(L) - 1)
                xdst = self.out if last else self.xs[li % 2]
                if layer % 2 == 0:
                    self.layer_A(j)
                    wout, pgbsrc, wgate, goff = self.a_w_out[j], self.a_pgb[j], self.a_w_in[j], 3584
                else:
                    self.layer_B(j)
                    wout, pgbsrc, wgate, goff = self.b_w_out[j], self.b_pgb[j], self.b_w_in[j], 2888
                S.barrier()
                if DEBUG and li == 0:
                    self.DMA(self.dbg[:, :], self.QY[:, :, :].rearrange("p f t -> p (f t)"), "dbg", r=[("QY", f, t) for f in range(16) for t in range(16)], w=[])
                    self.DMA(self.dbg_k[:, :], self.kTm[:, :, :].rearrange("p f t -> p (f t)"), "dbg", r=[("kTm", h) for h in range(4)], w=[])
                    self.DMA(self.dbg_v[:, :], self.vm[:, :, :].rearrange("p f t -> p (f t)"), "dbg", r=[("vm", 0, 0)], w=[])
                    self.DMA(self.dbg_m[:, :], self.memT[:, :, :].rearrange("p f t -> p (f t)"), "dbg", r=[("memT", 0)], w=[])
                    S.barrier()
                self.gate_out(wgate, goff, wout, pgbsrc, xsrc, xdst, last, li)
                S.barrier()
                xsrc = xdst
            S.emit(nc, st)
        return nc

    def layernorm_tile(self, s):
        xk = ("xio", s)
        x = self.xio[s]
        lnst, lnag, pgb, xb = self.lnst, self.lnag, self.pgb, self.xb[s]
        S = self.S
        for c in range(2):
            S.op("dve", (lambda e, c=c, lnst=lnst, x=x: e.bn_stats(lnst[:, c * 6:(c + 1) * 6], x[:, c * 512:(c + 1) * 512])),
                 r=[xk], w=[("lnst", c)])
        S.op("dve", (lambda e, lnst=lnst, lnag=lnag: e.bn_aggr(lnag[:, 0:2], lnst[:, :])), r=[("lnst", 0), ("lnst", 1)], w=["lnag"])
        self.TS("dve", lnag[:, 2:3], lnag[:, 1:2], LN_EPS, None, ALU.add, None, r=["lnag"], w=["lnrs0"])
        S.op("act", (lambda e, lnag=lnag: e.sqrt(lnag[:, 2:3], lnag[:, 2:3])), r=["lnrs0"], w=["lnrs0"])
        S.op("dve", (lambda e, lnag=lnag: e.reciprocal(lnag[:, 3:4], lnag[:, 2:3])), r=["lnrs0"], w=["lnrs"])
        self.TS("dve", x[:], x[:], lnag[:, 0:1], lnag[:, 3:4], ALU.subtract, ALU.mult, r=[xk, "lnag", "lnrs"], w=[xk])
        self.TT("dve", x[:], x[:], pgb[:, 0:D_MODEL], ALU.mult, r=[xk, "pgb"], w=[xk])
        self.TT("dve", x[:], x[:], pgb[:, D_MODEL:2 * D_MODEL], ALU.add, r=[xk, "pgb"], w=[xk])
        self.CP("act", xb[:], x[:], r=[xk], w=[("xb", s)])

    def to_T(self, s, dstT, t, dkey):
        pi = self.psum()
        pk = ("ps", pi)
        pv = self.PS[pi][:].bitcast(BF16)
        for k in range(8):
            self.TR(pv[:, k * 128:(k + 1) * 128], self.xb[s][:, k * 128:(k + 1) * 128], self.ident[:],
                    r=[("xb", s), "ident"], w=[pk])
        self.CP("dve", dstT[:, :, t * 128:(t + 1) * 128], pv.rearrange("p (k t) -> p k t", k=8), r=[pk], w=[dkey])

    def proj_fm(self, wbfv, wkey, ct, tb, evac):
        pi = self.psum()
        pk = ("ps", pi)
        for k in range(8):
            self.MM(self.PS[pi][:, :], wbfv[:, k, ct * 128:(ct + 1) * 128], self.xT[:, k, tb * 512:(tb + 1) * 512],
                    k == 0, k == 7, r=[wkey, ("xT", tb)], w=[pk])
        evac(pi, pk)

    def win_pieces(self, w2d, cols):
        pieces = []
        off = 0
        for (c0, n) in cols:
            src = w2d[:, c0:c0 + n].rearrange("(k p) c -> p k c", p=128)
            pieces.append(((lambda v, off=off, n=n: v[:, :, off:off + n]), src))
            off += n
        return pieces, off

    def mem_kv(self, wmkv):
        for g in range(2):
            pieces, n = self.win_pieces(wmkv, [(g * 256, 256)])
            wv, wk = self.wload(pieces, [128, 8, 256])
            for ct in range(2):
                h = g * 2 + ct
                pi = self.psum(); pk = ("ps", pi)
                for k in range(8):
                    self.MM(self.PS[pi][:, 0:N_MEM], wv[:, k, ct * 128:(ct + 1) * 128], self.memT[:, k, :], k == 0, k == 7,
                            r=[wk, ("memT", 0), ("memT", 1)], w=[pk])
                self.CP("act", self.kTm[:, h, :], self.PS[pi][:, 0:N_MEM], r=[pk], w=[("kTm", h)])
        for g in range(2):
            pieces, n = self.win_pieces(wmkv, [(512 + g * 256, 256)])
            wv, wk = self.wload(pieces, [128, 8, 256])
            for c in range(2):
                pi = self.psum(); pk = ("ps", pi)
                for k in range(8):
                    self.MM(self.PS[pi][:, 0:256], self.memT[:, k, c * 128:(c + 1) * 128], wv[:, k, :], k == 0, k == 7,
                            r=[wk, ("memT", c)], w=[pk])
                self.CP("act", self.vm[:, c, g * 256:(g + 1) * 256], self.PS[pi][:, 0:256], r=[pk], w=[("vm", c, g)])

    def _ak(self, slot, scr):
        sm = scr["sm"]
        sid = (scr["id"], slot)
        c0s = 16 + 8 * slot
        cols = [sm[:, c0s + i:c0s + i + 1] for i in range(4)]
        keys = [("smc", sid, i) for i in range(4)]
        return sid, cols, keys

    def at_qk(self, job, slot, scr):
        qT, qkeys, kT, kkeys, nk, bias = job["qT"], job["qkeys"], job["kT"], job["kkeys"], job["nk"], job["bias"]
        sid, cols, keys = self._ak(slot, scr)
        nch = (nk + 511) // 512
        if bias is not None:
            Dsl, ch, M, mkeys = bias
            z = scr["z"][slot]
            zk = ("z", sid)
            for c in range(nch):
                c0 = c * 512
                n = min(512, nk - c0)
                pi = self.psum(); pk = ("ps", pi)
                self.MM(self.PS[pi][:, 0:n], self.ident[:, :], M[:, c0:c0 + n], True, False, r=["ident"] + list(mkeys), w=[pk])
                self.MM(self.PS[pi][:, 0:n], qT, kT[:, c0:c0 + n], False, True, r=list(qkeys) + list(kkeys), w=[pk])
                self.STT("dve", z[:, c0:c0 + n], Dsl[:, c0:c0 + n], ch, self.PS[pi][:, 0:n], ALU.mult, ALU.add,
                         r=[pk, "dtab"], w=[zk])
        else:
            pi = self.psum(); pk = ("ps", pi)
            self.MM(self.PS[pi][:, 0:nk], qT, kT[:, 0:nk], True, True, r=list(qkeys) + list(kkeys), w=[pk])
            job["_ps"] = pi

    def at_max(self, job, slot, scr):
        nk = job["nk"]
        sid, (cmx, cnm, crs, cri), (kmx, knm, krs, kri) = self._ak(slot, scr)
        p = scr["p"][slot]
        pkey = ("p", sid)
        if job["bias"] is not None:
            src, sk = scr["z"][slot][:, 0:nk], ("z", sid)
        else:
            src, sk = self.PS[job["_ps"]][:, 0:nk], ("ps", job["_ps"])
        self.TS("dve", p[:, 0:nk], src, -SCALE, None, ALU.mult, ALU.min, r=[sk], w=[pkey, kmx, knm], accum=cnm)

    def at_exp(self, job, slot, scr):
        nk = job["nk"]
        sid, (cmx, cnm, crs, cri), (kmx, knm, krs, kri) = self._ak(slot, scr)
        p = scr["p"][slot]
        pkey = ("p", sid)
        if job["bias"] is not None:
            src, sk = scr["z"][slot][:, 0:nk], ("z", sid)
        else:
            src, sk = self.PS[job["_ps"]][:, 0:nk], ("ps", job["_ps"])
        self.ACT(p[:, 0:nk], src, AF.Exp, r=[sk, knm], w=[pkey, krs], bias=cnm, scale=SCALE, accum=crs)

    def at_tr(self, job, slot, scr):
        nk = job["nk"]
        sid, cols, keys = self._ak(slot, scr)
        p = scr["p"][slot]
        pkey = ("p", sid)
        nc128 = nk // 128
        banks = []
        for b0 in range(0, nc128, 8):
            nb = min(8, nc128 - b0)
            pi = self.psum(); pk = ("ps", pi)
            pv = self.PS[pi][:].bitcast(BF16)
            for c in range(nb):
                self.TR(pv[:, c * 128:(c + 1) * 128], p[:, (b0 + c) * 128:(b0 + c + 1) * 128], self.ident[:],
                        r=[pkey, "ident"], w=[pk])
            banks.append((b0, nb, pi))
        job["_tb"] = banks

    def at_evac(self, job, slot, scr):
        pT = scr["pT"]
        ptk = ("pT", scr["id"])
        for (b0, nb, pi) in job["_tb"]:
            pv = self.PS[pi][:].bitcast(BF16)
            self.CP("act", pT[:, b0:b0 + nb, :], pv[:, 0:nb * 128].rearrange("p (c t) -> p c t", c=nb), r=[("ps", pi)], w=[(ptk, b0)])

    def at_pv(self, job, slot, scr):
        nk, vfn, vkeys = job["nk"], job["vfn"], job["vkeys"]
        pT = scr["pT"]
        ptk = ("pT", scr["id"])
        nc128 = nk // 128
        pi = self.psum(); pk = ("ps", pi)
        for c in range(nc128):
            self.MM(self.PS[pi][:, 0:128], pT[:, c, :], vfn(c), c == 0, c == nc128 - 1,
                    r=[(ptk, (c // 8) * 8)] + list(vkeys), w=[pk])
        job["_pv"] = pi

    def at_fin(self, job, slot, scr):
        sid, (cmx, cnm, crs, cri), (kmx, knm, krs, kri) = self._ak(slot, scr)
        self.S.op("dve", (lambda e, cri=cri, crs=crs: e.reciprocal(cri, crs)), r=[krs], w=[kri])
        pi = job["_pv"]
        self.TS("dve", job["ydst"], self.PS[pi][:, 0:128], cri, None, ALU.mult, None, r=[("ps", pi), kri], w=[job["ydkey"]])
        if job.get("post") is not None:
            job["post"]()

    def attn_pipeline_gen(self, jobs, scr):
        n = len(jobs)
        for i in range(n + 1):
            cur = jobs[i] if i < n else None
            prv = jobs[i - 1] if i >= 1 else None
            cs, ps_ = i % 2, (i - 1) % 2
            if cur is not None:
                self.at_qk(cur, cs, scr)
            if prv is not None:
                self.at_tr(prv, ps_, scr)
                self.at_evac(prv, ps_, scr)
            if cur is not None:
                self.at_max(cur, cs, scr)
                self.at_exp(cur, cs, scr)
            if prv is not None:
                self.at_pv(prv, ps_, scr)
                self.at_fin(prv, ps_, scr)
            yield

    def attn_pipeline(self, jobs, scr, side=None, per=1):
        for _ in self.attn_pipeline_gen(jobs, scr):
            if side is not None:
                for _k in range(per):
                    next(side, None)

    def ytok_to_QY(self, ytok, ykeys, f0, nf, qt):
        for b0 in range(0, nf, 8):
            nb = min(8, nf - b0)
            pi = self.psum(); pk = ("ps", pi)
            pv = self.PS[pi][:].bitcast(BF16)
            for c in range(nb):
                self.TR(pv[:, c * 128:(c + 1) * 128], ytok[:, (b0 + c) * 128:(b0 + c + 1) * 128], self.ident[:],
                        r=list(ykeys) + ["ident"], w=[pk])
            self.CP("act", self.QY[:, f0 + b0:f0 + b0 + nb, qt * 128:(qt + 1) * 128],
                    pv[:, 0:nb * 128].rearrange("p (c t) -> p c t", c=nb), r=[pk],
                    w=[("QY", f0 + b0 + c, qt) for c in range(nb)])

    def mem_attention(self, scr, ytok):
        jobs = []
        for qt in range(16):
            for h in range(4):
                job = dict(qT=self.QY[:, 12 + h, qt * 128:(qt + 1) * 128], qkeys=[("QY", 12 + h, qt)],
                           kT=self.kTm[:, h, :], kkeys=[("kTm", h)], nk=N_MEM,
                           vfn=(lambda c, h=h: self.vm[:, c, h * 128:(h + 1) * 128]),
                           vkeys=[("vm", 0, 0), ("vm", 0, 1), ("vm", 1, 0), ("vm", 1, 1)],
                           bias=None, ydst=ytok[:, h * 128:(h + 1) * 128], ydkey=("ytok", h), post=None)
                if h == 3:
                    job["post"] = (lambda qt=qt: self.ytok_to_QY(ytok, [("ytok", hh) for hh in range(4)], 12, 4, qt))
                jobs.append(job)
        return self.attn_pipeline_gen(jobs, scr)

    def layer_A(self, j):
        nc = self.nc
        S = self.S
        w_in = self.a_w_in[j]
        with ExitStack() as st:
            sb = lambda name, shape, dtype: st.enter_context(nc.sbuf_tensor(self.nm(name), shape, dtype))
            hbuf = [sb("h%d" % i, [128, 2080], BF16) for i in range(2)]
            dg = [sb("dg%d" % i, [128, CONV_K, 128], BF16) for i in range(2)]
            sig = [sb("sig%d" % i, [128, 512], F32) for i in range(2)]
            spA = sb("spA", [128, 36 + 12 * CONV_K], F32)
            stt = [sb("stt%d" % i, [128, 512], F32) for i in range(4)]
            sq = [sb("sq%d" % i, [128, 512], BF16) for i in range(2)]
            scr = {"id": "A", "p": [sb("pA%d" % i, [128, 256], BF16) for i in range(2)],
                   "pT": sb("pTA", [128, 2, 128], BF16), "sm": self.sm}
            ytok = sb("ytokA", [128, 512], BF16)

            self.DMA(spA[:], self.a_sp[j], "spA", r=[], w=["spA"])
            for i in range(2):
                self.MEMSET("dve", hbuf[i][:, 0:32], 0.0, w=[("h", i)])
            self.mem_kv(self.a_w_mkv[j])

            for g in range(2):
                pieces, n = self.win_pieces(w_in, [(3072 + g * 256, 256)])
                wv, wk = self.wload(pieces, [128, 8, 256])
                for ct in range(2):
                    f = 12 + g * 2 + ct
                    for tb in range(4):
                        def ev(pi, pk, f=f, tb=tb):
                            self.CP("act", self.QY[:, f, tb * 512:(tb + 1) * 512], self.PS[pi][:, :], r=[pk],
                                    w=[("QY", f, tb * 4 + q) for q in range(4)])
                        self.proj_fm(wv, wk, ct, tb, ev)

            mgen = self.mem_attention(scr, ytok)
            for c in range(12):
                hs = c % 2
                hk = ("h", hs)
                pieces, n = self.win_pieces(w_in, [(c * 128, 128), (1536 + c * 128, 128)])
                wv, wk = self.wload(pieces, [128, 8, 256])
                self.TT("dve", dg[hs][:, :, :],
                        self.identf[:, :].unsqueeze(1).to_broadcast([128, CONV_K, 128]),
                        spA[:, 36 + c * CONV_K:36 + (c + 1) * CONV_K].unsqueeze(2).to_broadcast([128, CONV_K, 128]),
                        ALU.mult, r=["identf", "spA"], w=[("dg", hs)])
                for tb in range(4):
                    ss = tb % 2
                    def ev_g(pi, pk, ss=ss):
                        self.ACT(sig[ss][:, :], self.PS[pi][:, :], AF.Sigmoid, r=[pk], w=[("sig", ss)])
                    def ev_a(pi, pk, ss=ss, tb=tb, hs=hs, hk=hk):
                        self.TT("dve", hbuf[hs][:, 30 + tb * 512:30 + (tb + 1) * 512], self.PS[pi][:, :], sig[ss][:, :],
                                ALU.mult, r=[pk, ("sig", ss)], w=[(hk, tb)])
                    self.proj_fm(wv, wk, 1, tb, ev_g)
                    self.proj_fm(wv, wk, 0, tb, ev_a)
                for tb in range(4):
                    pi = self.psum(); pk = ("ps", pi)
                    rk = [hk, ("dg", hs)] + [(hk, t2) for t2 in range(max(0, tb - 1), tb + 1)]
                    for k in range(CONV_K):
                        self.MM(self.PS[pi][:, :], dg[hs][:, k, :], hbuf[hs][:, tb * 512 + k:tb * 512 + k + 512],
                                k == 0, k == CONV_K - 1, r=rk, w=[pk])
                    self.ACT(self.QY[:, c, tb * 512:(tb + 1) * 512], self.PS[pi][:, :], AF.Identity, r=[pk, "spA"],
                             w=[("QY", c, tb * 4 + q) for q in range(4)], bias=spA[:, c:c + 1], scale=1.0)
            for _ in mgen:
                pass

            for tb in range(4):
                tsl = slice(tb * 512, (tb + 1) * 512)
                p1 = self.psum(); p2 = self.psum()
                for c in range(12):
                    qk = [("QY", c, tb * 4 + q) for q in range(4)]
                    ss = c % 2
                    self.ACT(sq[ss][:, :], self.QY[:, c, tsl], AF.Square, r=qk, w=[("sq", ss)])
                    self.MM(self.PS[p1][:, :], self.ones[:, :], self.QY[:, c, tsl], c == 0, c == 11, r=qk + ["ones"], w=[("ps", p1)])
                    self.MM(self.PS[p2][:, :], self.ones[:, :], sq[ss][:, :], c == 0, c == 11, r=[("sq", ss), "ones"], w=[("ps", p2)])
                mean, msq, var, rstd = stt
                self.ACT(mean[:, :], self.PS[p1][:, :], AF.Identity, r=[("ps", p1)], w=["stt0"], scale=1.0 / CONV_W)
                self.TT("dve", msq[:, :], mean[:, :], mean[:, :], ALU.mult, r=["stt0"], w=["stt1"])
                self.STT("dve", var[:, :], self.PS[p2][:, :], 1.0 / CONV_W, msq[:, :], ALU.mult, ALU.subtract,
                         r=[("ps", p2), "stt1"], w=["stt2"])
                self.TS("dve", var[:, :], var[:, :], LN_EPS, None, ALU.add, None, r=["stt2"], w=["stt2"])
                self.S.op("act", lambda e, var=var: e.sqrt(var[:, :], var[:, :]), r=["stt2"], w=["stt2"])
                self.S.op("dve", lambda e, var=var, rstd=rstd: e.reciprocal(rstd[:, :], var[:, :]), r=["stt2"], w=["stt3"])
                for c in range(12):
                    qk = [("QY", c, tb * 4 + q) for q in range(4)]
                    ss = c % 2
                    self.TT("dve", sig[ss][:, :], self.QY[:, c, tsl], mean[:, :], ALU.subtract, r=qk + ["stt0"], w=[("sig", ss)])
                    self.TT("dve", sig[ss][:, :], sig[ss][:, :], rstd[:, :], ALU.mult, r=[("sig", ss), "stt3"], w=[("sig", ss)])
                    self.ACT(self.QY[:, c, tsl], sig[ss][:, :], AF.Silu, r=[("sig", ss), "spA"], w=qk,
                             bias=spA[:, 24 + c:25 + c], scale=spA[:, 12 + c:13 + c])

    def layer_B(self, j):
        nc = self.nc
        S = self.S
        w_in = self.b_w_in[j]
        slopes = _alibi_slopes(ATT_H)
        with ExitStack() as st:
            sb = lambda name, shape, dtype: st.enter_context(nc.sbuf_tensor(self.nm(name), shape, dtype))
            kT = sb("kT", [128, SEQ], BF16)
            v = sb("v", [128, 16, 128], BF16)
            qiT = sb("qiT", [128, 4, SEQ], BF16)
            kiT2 = sb("kiT2", [128, SEQ], BF16)
            wi = sb("wi", [128, 16, 8], F32)
            dtab = sb("dtab", [128, SEQ], F32)
            madm = sb("madm", [128, 256], F32)
            zz = [sb("zB%d" % i, [128, SEQ], F32) for i in range(2)]
            z = zz[0]
            M = sb("MB", [128, SEQ], BF16)
            pp = [sb("pB%d" % i, [128, SEQ], BF16) for i in range(2)]
            junk = pp[0]
            pT = sb("pTB", [128, 16, 128], BF16)
            ytok = sb("ytokB", [128, CONV_W], BF16)
            rl = [sb("rl%d" % i, [128, 512], F32) for i in range(2)]
            bis = sb("bis", [128, 64], F32)
            for it in range(NBISECT):
                self.MEMSET("dve", bis[:, it:it + 1], 2.0 ** (-(it + 1)), w=["cnc"])
            sm = self.sm
            scr = {"id": "B", "z": zz, "p": pp, "pT": pT, "sm": sm}
            zk = ("z", ("B", 0))
            jk = ("p", ("B", 0))

            self.DMA(dtab[:], self.c_dtab[:, :], "cst", r=[], w=["dtab"])
            self.DMA(madm[:], self.c_madm[:, :], "cst", r=[], w=["madm"])

            def fm_group(cols, dests, side=None):
                pieces, n = self.win_pieces(w_in, cols)
                wv, wk = self.wload(pieces, [128, 8, n])
                for ct, (dst_fn, key_fn) in enumerate(dests):
                    for tb in range(4):
                        def ev(pi, pk, dst_fn=dst_fn, key_fn=key_fn, tb=tb):
                            self.CP("act", dst_fn(tb), self.PS[pi][:, :], r=[pk], w=key_fn(tb))
                        self.proj_fm(wv, wk, ct, tb, ev)
                        if side is not None:
                            next(side, None)
                            next(side, None)

            def qy_dst(f):
                return ((lambda tb: self.QY[:, f, tb * 512:(tb + 1) * 512]),
                        (lambda tb: [("QY", f, tb * 4 + q) for q in range(4)]))

            fm_group([(1536, 128), (2304, 64), (2304, 64)],
                     [((lambda tb: kT[:, tb * 512:(tb + 1) * 512]), (lambda tb: [("kT", tb)])),
                      ((lambda tb: kiT2[:, tb * 512:(tb + 1) * 512]), (lambda tb: [("kiT2", tb)]))])
            pieces, n = self.win_pieces(w_in, [(1664, 128), (2368, 8)])
            wv, wk = self.wload(pieces, [128, 8, 136])
            for t in range(16):
                pi = self.psum(); pk = ("ps", pi)
                for k in range(8):
                    self.MM(self.PS[pi][:, 0:136], self.xT[:, k, t * 128:(t + 1) * 128], wv[:, k, :], k == 0, k == 7,
                            r=[wk, ("xT", t // 4)], w=[pk])
                self.CP("act", v[:, t, :], self.PS[pi][:, 0:128], r=[pk], w=[("v", t)])
                self.CP("act", wi[:, t, :], self.PS[pi][:, 128:136], r=[pk], w=[("wi", t)])
            for g in range(2):
                fm_group([(1792 + g * 256, 256)],
                         [((lambda tb, f=g * 2 + c: qiT[:, f, tb * 512:(tb + 1) * 512]),
                           (lambda tb, f=g * 2 + c: [("qiT", f, tb)])) for c in range(2)])
            for g in range(2):
                fm_group([(2376 + g * 256, 256)], [qy_dst(12 + g * 2), qy_dst(12 + g * 2 + 1)])
            self.mem_kv(self.b_w_mkv[j])
            mgen = self.mem_attention(scr, ytok)
            for g in range(6):
                fm_group([(g * 256, 256)], [qy_dst(g * 2), qy_dst(g * 2 + 1)])
            for _ in mgen:
                pass

            cA, cLO, cW, cMID, cCNT, cG = 8, 9, 10, 11, 12, 13
            col = lambda c: sm[:, c:c + 1]
            ck = lambda c: ("smb", c)
            isc = self.wst[1]
            isck = ("wst", 1)
            junk = self.wbf[0]
            jk = ("wbf", 0)
            Mb = [M, self.wbf[1]]
            Mk = [[("M", 0)], [("M", 1), ("wbf", 1)]]

            def mask_gen(qt):
                nk = 128 * (qt + 1)
                nch = (nk + 511) // 512
                Mq = Mb[qt % 2]
                mk = Mk[qt % 2]
                if qt < 2:
                    if nk > 128:
                        self.MEMSET("dve", Mq[:, 0:nk - 128], 0.0, w=mk)
                    self.CP("dve", Mq[:, nk - 128:nk], madm[:, 0:128], r=["madm"], w=mk)
                    yield
                    return
                for h in range(IDX_H):
                    hb = (h % 2) * 64
                    for c in range(nch):
                        c0 = c * 512
                        n = min(512, nk - c0)
                        pi = self.psum(); pk = ("ps", pi)
                        ss = (h * nch + c) % 2
                        self.MM(self.PS[pi][:, 0:n], qiT[hb:hb + 64, h // 2, qt * 128:(qt + 1) * 128], kiT2[hb:hb + 64, c0:c0 + n],
                                True, True, r=[("qiT", h // 2, qt // 4), ("kiT2", c)], w=[pk])
                        self.ACT(rl[ss][:, 0:n], self.PS[pi][:, 0:n], AF.Relu, r=[pk], w=[("rl", ss)])
                        if h == 0:
                            self.TS("dve", isc[:, c0:c0 + n], rl[ss][:, 0:n], wi[:, qt, 0:1], None, ALU.mult, None,
                                    r=[("rl", ss), ("wi", qt)], w=[isck])
                        else:
                            self.STT("dve", isc[:, c0:c0 + n], rl[ss][:, 0:n], wi[:, qt, h:h + 1], isc[:, c0:c0 + n], ALU.mult, ALU.add,
                                     r=[("rl", ss), ("wi", qt), isck], w=[isck])
                        yield
                self.TS("dve", junk[:, 0:nk], isc[:, 0:nk], 0.0, None, ALU.add, ALU.max, r=[isck], w=[jk, ck(cA)], accum=col(cA))
                self.TS("dve", junk[:, 0:nk], isc[:, 0:nk], 0.0, None, ALU.add, ALU.min, r=[isck], w=[jk, ck(cLO)], accum=col(cLO))
                yield
                self.TT("dve", col(cW), col(cA), col(cLO), ALU.subtract, r=[ck(cA), ck(cLO)], w=[ck(cW)])
                self.TS("dve", col(cW), col(cW), 1.000002, 1e-20, ALU.mult, ALU.add, r=[ck(cW)], w=[ck(cW)])
                self.TT("dve", isc[:, nk - 128:nk], isc[:, nk - 128:nk], madm[:, 128:256], ALU.add, r=[isck, "madm"], w=[isck])
                self.TS("dve", bis[:, 32:32 + NBISECT], bis[:, 0:NBISECT], col(cW), None, ALU.mult, None, r=["cnc", ck(cW)], w=["wn"])
                self.TT("dve", col(cMID), col(cLO), bis[:, 32:33], ALU.add, r=[ck(cLO), "wn"], w=[ck(cMID)])
                yield
                for it in range(NBISECT):
                    lastit = (it == NBISECT - 1)
                    self.TS("dve", junk[:, 0:nk], isc[:, 0:nk], col(cMID), None, ALU.is_ge, ALU.add, r=[isck, ck(cMID)],
                            w=[jk, ck(cCNT)], accum=col(cCNT))
                    self.TS("dve", col(cG), col(cCNT), float(TOPK) - 0.5, (1.0 if lastit else 0.5), ALU.is_ge, ALU.subtract,
                            r=[ck(cCNT)], w=[ck(cG)])
                    dst, dk = (col(cLO), ck(cLO)) if lastit else (col(cMID), ck(cMID))
                    self.STT("dve", dst, col(cG), bis[:, 32 + it:33 + it], col(cMID), ALU.mult, ALU.add,
                             r=[ck(cG), "wn", ck(cMID)], w=[dk])
                    yield
                self.TS("dve", Mq[:, 0:nk], isc[:, 0:nk], col(cLO), MASKNEG, ALU.is_lt, ALU.mult, r=[isck, ck(cLO)], w=mk)
                yield

            for _ in mask_gen(0):
                pass
            for qt in range(16):
                nk = 128 * (qt + 1)
                nch = (nk + 511) // 512
                d0 = 1920 - 128 * qt
                jobs = []
                for h in range(ATT_H):
                    jobs.append(dict(qT=self.QY[:, h, qt * 128:(qt + 1) * 128], qkeys=[("QY", h, qt)],
                                     kT=kT[:, 0:nk], kkeys=[("kT", c) for c in range(nch)], nk=nk,
                                     vfn=(lambda c: v[:, c, :]), vkeys=[("v", c) for c in range(nk // 128)],
                                     bias=(dtab[:, d0:d0 + nk], -slopes[h] / SCALE, Mb[qt % 2], [("M", qt % 2)]),
                                     ydst=ytok[:, h * 128:(h + 1) * 128], ydkey=("ytok", h), post=None))
                side = mask_gen(qt + 1) if qt < 15 else None
                self.attn_pipeline(jobs, scr, side=side, per=5)
                if side is not None:
                    for _ in side:
                        pass
                self.ytok_to_QY(ytok, [("ytok", h) for h in range(ATT_H)], 0, ATT_H, qt)

    def gate_out(self, wgate, goff, wout, pgbsrc, xsrc, xdst, last, li):
        nc = self.nc
        S = self.S
        with ExitStack() as st:
            sb = lambda name, shape, dtype: st.enter_context(nc.sbuf_tensor(self.nm(name), shape, dtype))
            woutb = sb("woutb", [128, 16, D_MODEL], BF16)
            self.xio = [sb("xio%d" % i, [128, D_MODEL], F32) for i in range(2)]
            self.xb = [sb("xb%d" % i, [128, D_MODEL], BF16) for i in range(2)]
            self.pgb = sb("pgb", [128, 2 * D_MODEL], F32)
            self.lnst = sb("lnst", [128, 12], F32)
            self.lnag = sb("lnag", [128, 4], F32)
            gt = [sb("gt%d" % i, [128, 512], BF16) for i in range(2)]
            self.DMA(self.pgb[:], pgbsrc, "pgb", r=[], w=["pgb"])
            for g in range(8):
                pieces, n = self.win_pieces(wgate, [(goff + g * 256, 256)])
                wv, wk = self.wload(pieces, [128, 8, 256])
                for ct in range(2):
                    f = g * 2 + ct
                    for tb in range(4):
                        def ev(pi, pk, f=f, tb=tb):
                            ss = tb % 2
                            qk = [("QY", f, tb * 4 + q) for q in range(4)]
                            self.ACT(gt[ss][:, :], self.PS[pi][:, :], AF.Silu, r=[pk], w=[("gt", ss)])
                            self.TT("dve", self.QY[:, f, tb * 512:(tb + 1) * 512], self.QY[:, f, tb * 512:(tb + 1) * 512],
                                    gt[ss][:, :], ALU.mult, r=qk + [("gt", ss)], w=qk)
                        self.proj_fm(wv, wk, ct, tb, ev)
                s = self.w_rr % 2
                self.w_rr += 1
                stv = self.wst[s][:, :].rearrange("p (a b) -> p a b", a=2)
                self.DMA(stv, wout[g * 256:(g + 1) * 256, :].rearrange("(k p) c -> p k c", p=128), "wst%d" % s, r=[], w=[("wst", s)])
                self.CP("pool", woutb[:, 2 * g:2 * g + 2, :], stv, r=[("wst", s)], w=[("wout", g)])
            for t in range(16):
                s = t % 2
                xk = ("xio", s)
                self.DMA(self.xio[s][:], xsrc[t * 128:(t + 1) * 128, :], "xio%d" % s, r=[("xdram", li - 1, t)], w=[xk])
                for half in range(2):
                    pi = self.psum(); pk = ("ps", pi)
                    for f in range(16):
                        self.MM(self.PS[pi][:, :], self.QY[:, f, t * 128:(t + 1) * 128], woutb[:, f, half * 512:(half + 1) * 512],
                                f == 0, f == 15, r=[("QY", f, t), ("wout", f // 2)], w=[pk])
                    self.STT("dve", self.xio[s][:, half * 512:(half + 1) * 512], self.xio[s][:, half * 512:(half + 1) * 512], ALPHA,
                             self.PS[pi][:, :], ALU.mult, ALU.add, r=[pk, xk], w=[xk])
                self.layernorm_tile(s)
                self.DMA(xdst[t * 128:(t + 1) * 128, :], self.xio[s][:], "xout%d" % s, r=[xk], w=[("xdram", li, t)])
                if not last:
                    self.to_T(s, self.xT, t, ("xT", t // 4))


_PROG_CACHE = {}


def _get_prog(layers):
    key = tuple(layers)
    if key not in _PROG_CACHE:
        p = Prog(layers)
        p.build()
        _PROG_CACHE[key] = p
    return _PROG_CACHE[key]


def _consts():
    ident = np.eye(128, dtype=np.float32)
    p = np.arange(128, dtype=np.float32)[:, None]
    jj = np.arange(SEQ, dtype=np.float32)[None, :]
    dtab = np.abs(p - (jj - 1920.0)).astype(np.float32)
    madm = np.zeros((128, 256), dtype=np.float32)
    madm[0:64, 64:128] = MASKNEG
    madm[0:64, 128 + 64:256] = -1e30
    return ident, dtab, madm


def _host_inputs(inp, b):
    f = lambda a: np.ascontiguousarray(np.asarray(a, dtype=np.float32))
    ident, dtab, madm = _consts()
    rep = lambda v: np.ascontiguousarray(np.broadcast_to(np.asarray(v, np.float32)[None, :], (128, v.shape[-1])))
    a_sp = np.zeros((2, 128, 36 + 12 * CONV_K), np.float32)
    for j in range(2):
        a_sp[j, :, 0:12] = np.asarray(inp["a_conv_b"][j]).reshape(12, 128).T
        a_sp[j, :, 12:24] = np.asarray(inp["a_ln_g"][j]).reshape(12, 128).T
        a_sp[j, :, 24:36] = np.asarray(inp["a_ln_b"][j]).reshape(12, 128).T
        cw = np.asarray(inp["a_conv_w"][j]).reshape(CONV_K, 12, 128)
        a_sp[j, :, 36:] = np.transpose(cw, (2, 1, 0)).reshape(128, 12 * CONV_K)
    a_pgb = np.stack([np.concatenate([rep(inp["a_post_g"][j]), rep(inp["a_post_b"][j])], axis=1) for j in range(2)])
    b_pgb = np.stack([np.concatenate([rep(inp["b_post_g"][j]), rep(inp["b_post_b"][j])], axis=1) for j in range(2)])
    memgb = np.concatenate([rep(np.asarray(inp["mem_ln_g"])), rep(np.asarray(inp["mem_ln_b"]))], axis=1)
    return {
        "x": f(inp["x"][b]), "mem": f(inp["mem"][b]), "memgb": f(memgb),
        "c_ident": ident, "c_dtab": dtab, "c_madm": madm,
        "a_w_in": f(inp["a_w_in"]), "a_sp": f(a_sp), "a_w_mkv": f(inp["a_w_mkv"]), "a_w_out": f(inp["a_w_out"]),
        "a_pgb": f(a_pgb),
        "b_w_in": f(inp["b_w_in"]), "b_w_mkv": f(inp["b_w_mkv"]), "b_w_out": f(inp["b_w_out"]), "b_pgb": f(b_pgb),
    }


def run_layers(inp, layers, cores):
    prog = _get_prog(layers)
    maps = [_host_inputs(inp, b) for b in cores]
    shared = {}
    for m in maps[1:]:
        for k in m:
            if k not in ("x", "mem"):
                m[k] = maps[0][k]
    res = run_bass_kernel_spmd(prog.nc, maps, core_ids=list(range(len(cores))))
    if DEBUG:
        global _DBG
        _DBG = [{k: np.asarray(r[k]) for k in ("dbg", "dbg_k", "dbg_v", "dbg_m")} for r in res.results]
    return np.stack([r["out"] for r in res.results], axis=0)


def kernel(**inputs):
    inp = {k: np.asarray(v) for k, v in inputs.items()}
    out = run_layers(inp, [0, 1, 2, 3], list(range(8)))
    return out.astype(np.float32)
```
